# Optimizing a Trainium2 kernel written in Bass

```python
import math
import jax, jax.numpy as jnp
from jax import lax
import numpy as np

D_MODEL = 1024
BATCH = 16
SEQ = 4096
DEPTH = 2

EPS = 1e-6
ROPE_THETA = 10000.0
BLOCK_Q = 128
RET_HEADS = 4
RET_DK = 64
RET_DV = 128
RET_CHUNK = 128
FOX_HEADS = 4
FOX_DH = 128
MLA_HEADS = 4
MLA_Q_RANK = 256
MLA_KV_RANK = 128
MLA_NOPE = 128
MLA_ROPE = 64
MLA_V = 128
N_BRANCH = 3
BRANCH_W = 512
N_GROUPS = 4
EXP_PER_GROUP = 8
N_EXPERTS = N_GROUPS * EXP_PER_GROUP
TOP_K = 2
D_EXPERT = 512
MOE_BLOCK = 128

IN_SIZES = (RET_HEADS * RET_DK, RET_HEADS * RET_DK, RET_HEADS * RET_DV, RET_HEADS * RET_DV,
            FOX_HEADS * FOX_DH, FOX_HEADS * FOX_DH, FOX_HEADS * FOX_DH, FOX_HEADS,
            MLA_Q_RANK, MLA_KV_RANK, MLA_ROPE)
IN_OFFSETS = tuple(sum(IN_SIZES[:i + 1]) for i in range(len(IN_SIZES) - 1))
D_IN = sum(IN_SIZES)

kernel_name = "hybrid_retention_fox_mla_hiermoe_adaln"


def rms_norm(x, g):
    xf = x.astype(jnp.float32)
    y = xf * lax.rsqrt(jnp.mean(xf * xf, axis=-1, keepdims=True) + EPS)
    return (y * g.astype(jnp.float32)).astype(x.dtype)


def modulate(h, shift, scale):
    return h * (1 + scale[:, None, :]) + shift[:, None, :]


def rope(x, positions):
    d = x.shape[-1]
    inv = ROPE_THETA ** (-jnp.arange(0, d, 2, dtype=jnp.float32) / d)
    ang = positions.astype(jnp.float32)[..., None] * inv
    cos = jnp.cos(ang)[:, :, None, :]
    sin = jnp.sin(ang)[:, :, None, :]
    xf = x.astype(jnp.float32)
    x1, x2 = xf[..., : d // 2], xf[..., d // 2:]
    return jnp.concatenate([x1 * cos - x2 * sin, x2 * cos + x1 * sin], axis=-1).astype(x.dtype)


def retention(q, k, v, g):
    B, S, H, dk = q.shape
    dv = v.shape[-1]
    C = RET_CHUNK
    N = S // C
    dt = q.dtype
    f32 = jnp.float32
    log_gamma = jnp.log1p(-jnp.exp2(-5.0 - jnp.arange(H, dtype=f32)))
    idx = jnp.arange(C, dtype=f32)
    rel = idx[:, None] - idx[None, :]
    dmask = jnp.where(rel >= 0, jnp.exp(log_gamma[:, None, None] * jnp.maximum(rel, 0.0)), 0.0)
    decay_in = jnp.exp(log_gamma[:, None] * (idx + 1.0))
    decay_out = jnp.exp(log_gamma[:, None] * (C - 1.0 - idx))
    decay_chunk = jnp.exp(log_gamma * C)
    qc = q.astype(f32).reshape(B, N, C, H, dk)
    kc = (k.astype(f32) * dk ** -0.5).reshape(B, N, C, H, dk)
    vc = v.astype(f32).reshape(B, N, C, H, dv)
    scores = jnp.einsum('bnchd,bnmhd->bhncm', qc, kc) * dmask[None, :, None]
    inner = jnp.einsum('bhncm,bnmhe->bnche', scores, vc)
    kv = jnp.einsum('bnmhd,hm,bnmhe->nbhde', kc, decay_out, vc)

    def step(state, kv_n):
        return state * decay_chunk[None, :, None, None] + kv_n, state

    _, s_prev = lax.scan(step, jnp.zeros((B, H, dk, dv), f32), kv)
    cross = jnp.einsum('bnchd,hc,nbhde->bnche', qc, decay_in, s_prev)
    o = (inner + cross).reshape(B, S, H, dv)
    mu = jnp.mean(o, axis=-1, keepdims=True)
    var = jnp.mean(jnp.square(o - mu), axis=-1, keepdims=True)
    o = ((o - mu) * lax.rsqrt(var + EPS)).reshape(B, S, H * dv)
    return (jax.nn.silu(g.astype(f32)) * o).astype(dt)


def blocked_causal_attention(q, k, v, log_f_cum=None):
    B, S, H, d = q.shape
    dv = v.shape[-1]
    NQ = S // BLOCK_Q
    scale = d ** -0.5
    kpos = jnp.arange(S)
    qb = q.reshape(B, NQ, BLOCK_Q, H, d).transpose(1, 0, 2, 3, 4)
    blk_ids = jnp.arange(NQ)
    use_forget = log_f_cum is not None
    if use_forget:
        fk = log_f_cum.transpose(0, 2, 1)
        fq = log_f_cum.reshape(B, NQ, BLOCK_Q, H).transpose(1, 0, 3, 2)
        xs = (qb, blk_ids, fq)
    else:
        xs = (qb, blk_ids)

    def body(args):
        qblk, bi = args[0], args[1]
        s = jnp.einsum('bqhd,bkhd->bhqk', qblk, k).astype(jnp.float32) * scale
        if use_forget:
            s = s + args[2][..., None] - fk[:, :, None, :]
        qpos = bi * BLOCK_Q + jnp.arange(BLOCK_Q)
        s = jnp.where(kpos[None, :] <= qpos[:, None], s, -jnp.inf)
        p = jax.nn.softmax(s, axis=-1)
        return jnp.einsum('bhqk,bkhe->bqhe', p.astype(v.dtype), v)

    out = lax.map(body, xs)
    return out.transpose(1, 0, 2, 3, 4).reshape(B, S, H * dv)


def token_mixer(h, positions, w_in, fox_fb, mla_q_norm_g, mla_wq_up, mla_kv_norm_g, mla_wkv_up,
                gate_w, gate_b, branch_w, out_w):
    B, S, D = h.shape
    proj = h @ w_in
    rq, rk, rv, rg, fq, fk, fv, ff, mq, mkv, mkr = jnp.split(proj, IN_OFFSETS, axis=-1)
    rq = rope(rq.reshape(B, S, RET_HEADS, RET_DK), positions)
    rk = rope(rk.reshape(B, S, RET_HEADS, RET_DK), positions)
    ya = retention(rq, rk, rv.reshape(B, S, RET_HEADS, RET_DV), rg)
    log_f = jax.nn.log_sigmoid((ff + fox_fb).astype(jnp.float32))
    f_cum = jnp.cumsum(log_f, axis=1)
    yb = blocked_causal_attention(fq.reshape(B, S, FOX_HEADS, FOX_DH),
                                  fk.reshape(B, S, FOX_HEADS, FOX_DH),
                                  fv.reshape(B, S, FOX_HEADS, FOX_DH), f_cum)
    qh = (rms_norm(mq, mla_q_norm_g) @ mla_wq_up).reshape(B, S, MLA_HEADS, MLA_NOPE + MLA_ROPE)
    q_nope, q_pe = qh[..., :MLA_NOPE], rope(qh[..., MLA_NOPE:], positions)
    kvh = (rms_norm(mkv, mla_kv_norm_g) @ mla_wkv_up).reshape(B, S, MLA_HEADS, MLA_NOPE + MLA_V)
    k_nope, v_c = kvh[..., :MLA_NOPE], kvh[..., MLA_NOPE:]
    k_pe = jnp.broadcast_to(rope(mkr[:, :, None, :], positions), (B, S, MLA_HEADS, MLA_ROPE))
    yc = blocked_causal_attention(jnp.concatenate([q_nope, q_pe], axis=-1),
                                  jnp.concatenate([k_nope, k_pe], axis=-1), v_c)
    gates = jax.nn.sigmoid((h @ gate_w + gate_b).astype(jnp.float32)).astype(h.dtype)
    gates = gates.reshape(B, S, N_BRANCH, D)
    merged = gates[:, :, 0] * (ya @ branch_w[0])
    merged = merged + gates[:, :, 1] * (yb @ branch_w[1])
    merged = merged + gates[:, :, 2] * (yc @ branch_w[2])
    return merged @ out_w


def hier_moe(h, w_grp, b_grp, w_exp, b_exp, w1, w3, w2):
    B, S, D = h.shape
    T = B * S
    f32 = jnp.float32
    t = h.reshape(T, D)
    tf = t.astype(f32)
    grp_p = jax.nn.softmax(tf @ w_grp.astype(f32) + b_grp.astype(f32), axis=-1)
    g_idx = jnp.argmax(grp_p, axis=-1)
    g_w = jnp.max(grp_p, axis=-1)
    exp_logits = (tf @ w_exp.astype(f32) + b_exp.astype(f32)).reshape(T, N_GROUPS, EXP_PER_GROUP)
    in_grp = jnp.take_along_axis(exp_logits, g_idx[:, None, None], axis=1)[:, 0]
    top_p, top_i = lax.top_k(jax.nn.softmax(in_grp, axis=-1), TOP_K)
    top_p = top_p / jnp.sum(top_p, axis=-1, keepdims=True)
    eid = (g_idx[:, None] * EXP_PER_GROUP + top_i).reshape(-1).astype(jnp.int32)
    wts = (g_w[:, None] * top_p).reshape(-1)
    tok = jnp.repeat(jnp.arange(T, dtype=jnp.int32), TOP_K)
    A = T * TOP_K
    order = jnp.argsort(eid)
    se, stok, sw = eid[order], tok[order], wts[order]
    counts = jnp.bincount(eid, length=N_EXPERTS)
    starts = jnp.cumsum(counts) - counts
    pcounts = (counts + MOE_BLOCK - 1) // MOE_BLOCK * MOE_BLOCK
    pends = jnp.cumsum(pcounts)
    pstarts = pends - pcounts
    dest = pstarts[se] + jnp.arange(A) - starts[se]
    n_blocks = -(-A // MOE_BLOCK) + N_EXPERTS
    P = n_blocks * MOE_BLOCK
    buf_tok = jnp.zeros((P,), jnp.int32).at[dest].set(stok)
    buf_w = jnp.zeros((P,), f32).at[dest].set(sw)
    blk_e = jnp.minimum(jnp.searchsorted(pends, jnp.arange(n_blocks) * MOE_BLOCK, side='right'),
                        N_EXPERTS - 1)

    def expert_block(args):
        tok_b, w_b, e = args
        xb = t[tok_b]
        hid = jax.nn.silu(xb @ w1[e]) * (xb @ w3[e])
        return (hid @ w2[e]) * w_b[:, None].astype(t.dtype)

    ys = lax.map(expert_block, (buf_tok.reshape(n_blocks, MOE_BLOCK),
                                buf_w.reshape(n_blocks, MOE_BLOCK), blk_e))
    out = jnp.zeros((T, D), t.dtype).at[buf_tok].add(ys.reshape(P, D))
    return out.reshape(B, S, D)


def setup_inputs(seed: int = 0) -> dict:
    key = jax.random.key(seed)
    ks = jax.random.split(key, 26)
    L, D = DEPTH, D_MODEL
    f32 = jnp.float32

    def nrm(k, shape, fan_in, mult=1.0):
        return jax.random.normal(k, shape, f32) * (mult * fan_in ** -0.5)

    def gain(k, shape):
        return 1.0 + 0.05 * jax.random.normal(k, shape, f32)

    def small(k, shape, s=0.02):
        return s * jax.random.normal(k, shape, f32)

    positions = (jnp.arange(SEQ, dtype=jnp.int32)[None, :]
                 + jax.random.randint(ks[2], (BATCH, 1), 0, 1024, dtype=jnp.int32))
    return {
        "x": jax.random.normal(ks[0], (BATCH, SEQ, D), f32),
        "c": jax.random.normal(ks[1], (BATCH, D), f32),
        "positions": positions,
        "ada_w": nrm(ks[3], (L, D, 6 * D), D, 0.5),
        "ada_b": small(ks[4], (L, 6 * D)),
        "norm1_g": gain(ks[5], (L, D)),
        "norm2_g": gain(ks[6], (L, D)),
        "w_in": nrm(ks[7], (L, D, D_IN), D),
        "fox_fb": 1.0 + 0.5 * jax.random.normal(ks[8], (L, FOX_HEADS), f32),
        "mla_q_norm_g": gain(ks[9], (L, MLA_Q_RANK)),
        "mla_wq_up": nrm(ks[10], (L, MLA_Q_RANK, MLA_HEADS * (MLA_NOPE + MLA_ROPE)), MLA_Q_RANK),
        "mla_kv_norm_g": gain(ks[11], (L, MLA_KV_RANK)),
        "mla_wkv_up": nrm(ks[12], (L, MLA_KV_RANK, MLA_HEADS * (MLA_NOPE + MLA_V)), MLA_KV_RANK),
        "gate_w": nrm(ks[13], (L, D, N_BRANCH * D), D),
        "gate_b": small(ks[14], (L, N_BRANCH * D)),
        "branch_w": nrm(ks[15], (L, N_BRANCH, BRANCH_W, D), BRANCH_W),
        "out_w": nrm(ks[16], (L, D, D), D),
        "router_grp_w": nrm(ks[17], (L, D, N_GROUPS), D),
        "router_grp_b": small(ks[18], (L, N_GROUPS), 0.01),
        "router_exp_w": nrm(ks[19], (L, D, N_EXPERTS), D),
        "router_exp_b": small(ks[20], (L, N_EXPERTS), 0.01),
        "exp_w1": nrm(ks[21], (L, N_EXPERTS, D, D_EXPERT), D),
        "exp_w3": nrm(ks[22], (L, N_EXPERTS, D, D_EXPERT), D),
        "exp_w2": nrm(ks[23], (L, N_EXPERTS, D_EXPERT, D), D_EXPERT),
        "final_g": gain(ks[24], (D,)),
    }


def reference(x, c, positions, ada_w, ada_b, norm1_g, norm2_g, w_in, fox_fb, mla_q_norm_g,
              mla_wq_up, mla_kv_norm_g, mla_wkv_up, gate_w, gate_b, branch_w, out_w,
              router_grp_w, router_grp_b, router_exp_w, router_exp_b, exp_w1, exp_w3, exp_w2,
              final_g):
    c_act = jax.nn.silu(c)
    for l in range(DEPTH):
        mod = c_act @ ada_w[l] + ada_b[l]
        sh1, sc1, gt1, sh2, sc2, gt2 = jnp.split(mod, 6, axis=-1)
        h = modulate(rms_norm(x, norm1_g[l]), sh1, sc1)
        mix = token_mixer(h, positions, w_in[l], fox_fb[l], mla_q_norm_g[l], mla_wq_up[l],
                          mla_kv_norm_g[l], mla_wkv_up[l], gate_w[l], gate_b[l], branch_w[l], out_w[l])
        x = x + gt1[:, None, :] * mix
        h = modulate(rms_norm(x, norm2_g[l]), sh2, sc2)
        ffn = hier_moe(h, router_grp_w[l], router_grp_b[l], router_exp_w[l], router_exp_b[l],
                       exp_w1[l], exp_w3[l], exp_w2[l])
        x = x + gt2[:, None, :] * ffn
    return rms_norm(x, final_g)
```

```python
import contextlib
import math
import os
import numpy as np
import concourse.bass as bass
import concourse.mybir as mybir
from concourse.bass_utils import run_bass_kernel_spmd

F32 = mybir.dt.float32
BF16 = mybir.dt.bfloat16
I32 = mybir.dt.int32
AF = mybir.ActivationFunctionType
ALU = mybir.AluOpType
AX = mybir.AxisListType

NCORE = 8
L = 2
D = 1024
KC = 8
SEQ = 4096
NS = 2
T = NS * SEQ
G = 512
NG = T // G
NT = T // 128
NE = 32
NBLK = T * 2 // 128 + NE
NSLOT = NBLK * 128
EPS = 1e-6
WA_COLS = 2564
WB_COLS = 1536
SC_FOX = 128 ** -0.5
SC_MLA = 192 ** -0.5
SKIP_OFF = float(1 << 20)

C_ID = 0
C_TRI = 128
C_US = 256
C_DM = 384
C_DI = 896
C_DO = 1408
C_INVF = 1412
C_SGN = 1413
C_IOTA = 1414
C_THR = 1415
C_N = 1575

EPOCH = 30000
NDMA_SLOTS = 8


class Buf:
    __slots__ = ("ap", "lw", "rd", "excl")

    def __init__(self, ap=None, excl=False):
        self.ap = ap
        self.lw = None
        self.rd = {}
        self.excl = excl

    def __getitem__(self, idx):
        return self.ap[idx]


class Sched:
    def __init__(self, nc, stack, same_engine_sync=True):
        self.nc = nc
        self.stack = stack
        self.same = same_engine_sync
        self.engs = {"pe": nc.tensor, "act": nc.scalar, "dve": nc.vector, "pool": nc.gpsimd, "sp": nc.sync}
        self.cnt = {k: 0 for k in self.engs}
        self.esems = {k: [] for k in self.engs}
        self.waited = {k: {} for k in self.engs}
        self.dq = {}
        for q in ("sp", "act", "pool"):
            ns = NDMA_SLOTS
            sems = [self._newsem(f"dq_{q}_{i}") for i in range(ns)]
            self.dq[q] = {"sems": sems, "n": 0, "ns": ns}
        self.ninst = 0

    def _newsem(self, name):
        return self.stack.enter_context(self.nc.semaphore(name))

    def _esem(self, eng, epoch):
        lst = self.esems[eng]
        while len(lst) <= epoch:
            lst.append(self._newsem(f"e_{eng}_{len(lst)}"))
        return lst[epoch]

    def _wait(self, eng, sem, val):
        w = self.waited[eng]
        if w.get(id(sem), 0) >= val:
            return
        self.engs[eng].wait_ge(sem, val)
        w[id(sem)] = val

    def _deps(self, eng, reads, writes):
        toks = []
        for b in reads:
            if b.lw is not None:
                toks.append(b.lw)
        for b in writes:
            if b.lw is not None:
                toks.append(b.lw)
            toks.extend(b.rd.values())
        for t in toks:
            if t[2] == eng and (eng == "pe" or not self.same):
                continue
            self._wait(eng, t[0], t[1])

    def _mark(self, tok, reads, writes):
        for b in writes:
            b.lw = tok
            b.rd = {}
        for b in reads:
            if b not in writes:
                b.rd[id(tok[0])] = tok

    def op(self, eng, fn, reads=(), writes=()):
        ex = [b for b in reads if b.excl]
        if ex:
            writes = list(writes) + [b for b in ex if b not in writes]
            reads = [b for b in reads if not b.excl]
        self._deps(eng, reads, writes)
        n = self.cnt[eng]
        epoch, val = divmod(n, EPOCH)
        sem = self._esem(eng, epoch)
        inst = fn(self.engs[eng])
        inst.then_inc(sem, 1)
        self.cnt[eng] = n + 1
        self._mark((sem, val + 1, eng), reads, writes)
        self.ninst += 1
        return inst

    def dma(self, q, out, in_, reads=(), writes=(), fn=None, **kw):
        self._deps(q, reads, writes)
        st = self.dq[q]
        i = st["n"]
        ns = st["ns"]
        slot, rnd = i % ns, i // ns
        sem = st["sems"][slot]
        if rnd > 0:
            self._wait(q, sem, 16 * rnd)
        if fn is not None:
            inst = fn(self.engs[q])
        else:
            inst = self.engs[q].dma_start(out=out, in_=in_, **kw)
        inst.then_inc(sem, 16)
        st["n"] = i + 1
        self._mark((sem, 16 * (rnd + 1), "dma_" + q), reads, writes)
        self.ninst += 1
        return inst

    def barrier(self):
        toks = []
        for e, c in self.cnt.items():
            if c > 0:
                epoch, val = divmod(c - 1, EPOCH)
                toks.append((self.esems[e][epoch], val + 1))
        for q, st in self.dq.items():
            n = st["n"]
            ns = st["ns"]
            for slot in range(ns):
                issued = (n + ns - 1 - slot) // ns
                if issued > 0:
                    toks.append((st["sems"][slot], 16 * issued))
        for e in self.engs:
            for t in toks:
                self._wait(e, t[0], t[1])


def build(nlayers=L, dbg=(), stop_after=None):
    nc = bass.Bass("TRN2", target_bir_lowering=False)
    dbg = set(dbg)

    def din(name, shape, dt):
        return nc.dram_tensor(name, list(shape), dt, kind="ExternalInput").ap()

    def dscr(name, shape, dt):
        kind = "ExternalOutput" if name in dbg else "Internal"
        return nc.dram_tensor(name, list(shape), dt, kind=kind).ap()

    xT_in = din("xT", [KC, 128, T], F32)
    cT_in = din("cT", [128, KC, NS], F32)
    pos_in = din("pos", [1, T], I32)
    consts_in = din("consts", [128, C_N], F32)
    fg_in = din("fg", [128, KC], F32)
    W = []
    for l in range(L):
        W.append(dict(
            adaw=din(f"adaw{l}", [128, KC, 6 * D], F32), adab=din(f"adab{l}", [128, 48], F32),
            n1g=din(f"n1g{l}", [128, KC], F32), n2g=din(f"n2g{l}", [128, KC], F32),
            WA=din(f"WA{l}", [128, KC, WA_COLS], F32), WB=din(f"WB{l}", [128, KC, WB_COLS], F32),
            fb=din(f"fb{l}", [4, 1], F32), gq=din(f"gq{l}", [128, 2], F32), wq=din(f"wq{l}", [128, 2, 1024], F32),
            gkv=din(f"gkv{l}", [128, 1], F32), wkv=din(f"wkv{l}", [128, 1024], F32),
            Wg=din(f"Wg{l}", [128, KC, 3 * D], F32), gb=din(f"gb{l}", [128, 24], F32),
            Wbr=din(f"Wbr{l}", [128, 12, D], F32), Wo=din(f"Wo{l}", [128, KC, D], F32),
            Wr=din(f"Wr{l}", [128, KC, 36], F32), br=din(f"br{l}", [128, 36], F32),
            W13=din(f"W13{l}", [NE * 128 * 4, 2048], F32), W2=din(f"W2{l}", [NE * 128 * 2, 2048], F32),
        ))
    outT = nc.dram_tensor("outT", [KC, 128, T], F32, kind="ExternalOutput").ap()

    xw = dscr("xw", [KC, 128, T], F32)
    hTd = dscr("hTd", [KC, 128, T], BF16)
    fqT = dscr("fqT", [4, 128, T], BF16)
    fkT = dscr("fkT", [4, 128, T], BF16)
    fvd = dscr("fvd", [T, 512], BF16)
    F32d = dscr("F32d", [4, T], F32)
    F3d = dscr("F3d", [4, 3, T], BF16)
    mqnT = dscr("mqnT", [4, 128, T], BF16)
    mknT = dscr("mknT", [4, 128, T], BF16)
    mqpT = dscr("mqpT", [4, 64, T], BF16)
    kpeT = dscr("kpeT", [64, T], BF16)
    mvd = dscr("mvd", [T, 512], BF16)
    yTd = dscr("yTd", [3, 4, 128, T], BF16)
    h2tok = dscr("h2tok", [T, D], BF16)
    xs = dscr("xs", [NSLOT, D], BF16)
    Ys = dscr("Ys", [NSLOT, D], F32)
    rdbg = dscr("rdbg", [128, NT * 4], F32)
    Wcb = dscr("Wcb", [NE * 128, 12288], BF16)

    with contextlib.ExitStack() as top:
        S = Sched(nc, top)

        uid = [0]

        def sbt(stack, name, shape, dt):
            uid[0] += 1
            return Buf(stack.enter_context(nc.sbuf_tensor(f"sb{uid[0]}_{name}", list(shape), dt)))

        PS = [Buf(top.enter_context(nc.psum_tensor(f"ps{i}", [128, 512], F32)), excl=True) for i in range(8)]
        psrr = {}
        psrange = [0, 8]

        def nps(lo=None, hi=None):
            lo = psrange[0] if lo is None else lo
            hi = psrange[1] if hi is None else hi
            i = psrr.get((lo, hi), 0)
            psrr[(lo, hi)] = (i + 1) % (hi - lo)
            return PS[lo + i]

        r_slot = top.enter_context(nc.gpsimd.register("r_slot"))
        r_w13 = top.enter_context(nc.gpsimd.register("r_w13"))
        r_w2 = top.enter_context(nc.gpsimd.register("r_w2"))
        nc.gpsimd.reg_mov(r_slot, NSLOT - 1)
        nc.gpsimd.reg_mov(r_w13, NE * 128 * 4 - 1)
        nc.gpsimd.reg_mov(r_w2, NE * 128 * 2 - 1)
        r_e = top.enter_context(nc.gpsimd.register("r_e"))
        nc.gpsimd.reg_mov(r_e, NE * 128 - 1)
        cst = sbt(top, "cst", [128, C_N], F32)
        S.dma("sp", cst[:], consts_in, writes=[cst])
        identb = sbt(top, "identb", [128, 128], BF16)
        onesb = sbt(top, "onesb", [128, 128], BF16)
        negtri = sbt(top, "negtri", [128, 128], BF16)
        ustr = sbt(top, "ustr", [128, 128], BF16)
        epsc = sbt(top, "epsc", [128, 1], F32)
        onec = sbt(top, "onec", [128, 1], F32)
        ones512 = sbt(top, "ones512", [4, 512], F32)
        S.op("dve", lambda e: e.tensor_copy(identb[:], cst[:, C_ID:C_ID + 128]), reads=[cst], writes=[identb])
        S.op("dve", lambda e: e.memset(onesb[:], 1.0), writes=[onesb])
        S.op("dve", lambda e: e.tensor_copy(negtri[:], cst[:, C_TRI:C_TRI + 128]), reads=[cst], writes=[negtri])
        S.op("dve", lambda e: e.tensor_copy(ustr[:], cst[:, C_US:C_US + 128]), reads=[cst], writes=[ustr])
        S.op("dve", lambda e: e.memset(epsc[:], EPS), writes=[epsc])
        S.op("dve", lambda e: e.memset(onec[:], 1.0), writes=[onec])
        S.op("dve", lambda e: e.memset(ones512[:], 1.0), writes=[ones512])
        identf_ap = cst[:, C_ID:C_ID + 128]

        vec = [[sbt(top, f"vec{l}_{s}", [128, 6, KC], F32) for s in range(NS)] for l in range(L)]
        fgt = sbt(top, "fgt", [128, KC], F32)
        S.dma("sp", fgt[:], fg_in, writes=[fgt])

        with contextlib.ExitStack() as st:
            cT = sbt(st, "cT", [128, KC, NS], F32)
            cact = sbt(st, "cact", [128, KC, NS], F32)
            S.dma("sp", cT[:], cT_in, writes=[cT])
            S.op("act", lambda e: e.activation(cact[:], cT[:], AF.Silu), reads=[cT], writes=[cact])
            wblk = [sbt(st, f"wblk{i}", [128, KC, 512], F32) for i in range(2)]
            modt = sbt(st, "modt", [128, 48, NS], F32)
            adab = sbt(st, "adab", [128, 48], F32)
            ng = sbt(st, "ng", [128, 2, KC], F32)
            for l in range(nlayers):
                S.dma("sp", adab[:], W[l]["adab"], writes=[adab])
                S.dma("sp", ng[:, 0, :], W[l]["n1g"], writes=[ng])
                S.dma("sp", ng[:, 1, :], W[l]["n2g"], writes=[ng])
                mps = nps()
                for cb in range(12):
                    wb = wblk[cb % 2]
                    S.dma("sp" if cb % 2 == 0 else "act", wb[:], W[l]["adaw"][:, :, cb * 512:(cb + 1) * 512], writes=[wb])
                    for oc in range(4):
                        j = cb * 4 + oc
                        for kc in range(KC):
                            S.op("pe", lambda e: e.matmul(mps[:, j * 2:(j + 1) * 2], wb[:, kc, oc * 128:(oc + 1) * 128], cact[:, kc, :],
                                                          start=(kc == 0), stop=(kc == KC - 1)), reads=[wb, cact], writes=[mps])
                for s in range(NS):
                    S.op("dve", lambda e: e.tensor_tensor(modt[:, :, s], mps[:, 0:96].rearrange("p (j s) -> p j s", s=2)[:, :, s], adab[:], ALU.add),
                         reads=[mps, adab], writes=[modt])
                for s in range(NS):
                    v = vec[l][s]
                    S.op("dve", lambda e: e.scalar_tensor_tensor(v[:, 0, :], modt[:, 8:16, s], 1.0, ng[:, 0, :], ALU.add, ALU.mult), reads=[modt, ng], writes=[v])
                    S.op("dve", lambda e: e.tensor_copy(v[:, 1, :], modt[:, 0:8, s]), reads=[modt], writes=[v])
                    S.op("dve", lambda e: e.tensor_copy(v[:, 2, :], modt[:, 16:24, s]), reads=[modt], writes=[v])
                    S.op("dve", lambda e: e.scalar_tensor_tensor(v[:, 3, :], modt[:, 32:40, s], 1.0, ng[:, 1, :], ALU.add, ALU.mult), reads=[modt, ng], writes=[v])
                    S.op("dve", lambda e: e.tensor_copy(v[:, 4, :], modt[:, 24:32, s]), reads=[modt], writes=[v])
                    S.op("dve", lambda e: e.tensor_copy(v[:, 5, :], modt[:, 40:48, s]), reads=[modt], writes=[v])
            S.barrier()
        cut = int(os.environ.get("KCUT", "99"))
        if cut == 0:
            return nc

        def norm_mod(x32, n, sq, rstd, tmp, acol, bcol, out_bf=None, out_f32=None):
            S.op("act", lambda e: e.activation(sq[:, :, 0:n], x32[:, :, 0:n], AF.Square), reads=[x32], writes=[sq])
            ssp = nps()
            for kc in range(KC):
                S.op("pe", lambda e: e.matmul(ssp[:, 0:n], onesb[:], sq[:, kc, 0:n], start=(kc == 0), stop=(kc == KC - 1)),
                     reads=[onesb, sq], writes=[ssp])
            S.op("act", lambda e: e.activation(rstd[:, 0:n], ssp[:, 0:n], AF.Sqrt, bias=epsc[:], scale=1.0 / D), reads=[ssp, epsc], writes=[rstd])
            S.op("dve", lambda e: e.reciprocal(rstd[:, 0:n], rstd[:, 0:n]), reads=[rstd], writes=[rstd])
            for kc in range(KC):
                S.op("dve", lambda e: e.scalar_tensor_tensor(tmp[:, 0:n], x32[:, kc, 0:n], acol(kc), rstd[:, 0:n], ALU.mult, ALU.mult),
                     reads=[x32, rstd], writes=[tmp])
                if out_f32 is not None:
                    S.op("act", lambda e: e.activation(out_f32[:, kc, 0:n], tmp[:, 0:n], AF.Identity, bias=bcol(kc), scale=1.0), reads=[tmp], writes=[out_f32])
                    if out_bf is not None:
                        S.op("pool", lambda e: e.tensor_copy(out_bf[:, kc, 0:n], out_f32[:, kc, 0:n]), reads=[out_f32], writes=[out_bf])
                else:
                    S.op("act", lambda e: e.activation(out_bf[:, kc, 0:n], tmp[:, 0:n], AF.Identity, bias=bcol(kc), scale=1.0), reads=[tmp], writes=[out_bf])

        def wload(stack, name, shape, src, q="pool", defer=False, after=()):
            t = sbt(stack, name, shape, BF16)
            if defer:
                return t
            return wfill(t, shape, src, after)

        def wfill(t, shape, src, after=()):
            after = list(after)
            if len(shape) == 3:
                for kc in range(shape[1]):
                    c = 0
                    while c < shape[2]:
                        w = min(2048, shape[2] - c)
                        S.dma("pool", t[:, kc, c:c + w], src[:, kc, c:c + w], reads=after, writes=[t])
                        c += w
            else:
                c = 0
                while c < shape[1]:
                    w = min(2048, shape[1] - c)
                    S.dma("pool", t[:, c:c + w], src[:, c:c + w], reads=after, writes=[t])
                    c += w
            return t

        for l in range(nlayers):
            Wl = W[l]
            xsrc = xT_in if l == 0 else xw
            lst = contextlib.ExitStack()
            lst.__enter__()
            Wts = sbt(lst, f"Wts_{l}", [128, NT, 2], F32)
            d1v = sbt(lst, f"d1v_{l}", [128, NT], I32)
            d2v = sbt(lst, f"d2v_{l}", [128, NT], I32)
            ibv = sbt(lst, f"ibv_{l}", [128, NBLK], I32)
            with contextlib.ExitStack() as st:
                psrange[:] = [0, 6]
                WA = wload(st, "WA", [128, KC, WA_COLS], Wl["WA"])
                WB = wload(st, "WB", [128, KC, WB_COLS], Wl["WB"])
                wq = wload(st, "wq", [128, 2, 1024], Wl["wq"])
                wkv = wload(st, "wkv", [128, 1024], Wl["wkv"])
                gq = sbt(st, "gq", [128, 2], F32)
                gkv = sbt(st, "gkv", [128, 1], F32)
                nfb = sbt(st, "nfb", [4, 1], F32)
                S.dma("sp", gq[:], Wl["gq"], writes=[gq])
                S.dma("sp", gkv[:], Wl["gkv"], writes=[gkv])
                S.dma("sp", nfb[:], Wl["fb"], writes=[nfb])
                S.op("dve", lambda e: e.tensor_scalar(nfb[:], nfb[:], -1.0, None, ALU.mult), reads=[nfb], writes=[nfb])
                x32 = sbt(st, "x32", [128, KC, G], F32)
                sq = sbt(st, "sq", [128, KC, G], BF16)
                hT = sbt(st, "hT", [128, KC, G], BF16)
                rstd = sbt(st, "rstd", [128, G], F32)
                tmp = sbt(st, "tmp", [128, G], F32)
                posi = sbt(st, "posi", [128, G], I32)
                ang = sbt(st, "ang", [128, G], F32)
                rr = sbt(st, "rr", [128, G], F32)
                ki = sbt(st, "ki", [128, G], I32)
                kf = sbt(st, "kf", [128, G], F32)
                msk = sbt(st, "msk", [128, G], F32)
                cosT = sbt(st, "cosT", [128, G], F32)
                sinT = sbt(st, "sinT", [128, G], F32)
                t1 = [sbt(st, f"t1_{i}", [128, G], F32) for i in range(2)]
                t2 = [sbt(st, f"t2_{i}", [128, G], F32) for i in range(2)]
                R2q = [sbt(st, f"R2q{p}", [128, G], BF16) for p in range(2)]
                R2k = [sbt(st, f"R2k{p}", [128, G], BF16) for p in range(2)]
                rqO = [sbt(st, f"rqO{p}", [64, G], BF16) for p in range(2)]
                rkO = [sbt(st, f"rkO{p}", [64, G], BF16) for p in range(2)]
                rqB = [R2q[0], rqO[0], R2q[1], rqO[1]]
                rkB = [R2k[0], rkO[0], R2k[1], rkO[1]]

                def rqA(h, tsl):
                    return rqB[h][0:64, tsl]

                def rkA(h, tsl):
                    return rkB[h][0:64, tsl]
                stg = [sbt(st, f"stg{i}", [128, G], BF16) for i in range(3)]
                mq32 = sbt(st, "mq32", [128, 3, G], F32)
                msq = sbt(st, "msq", [128, 3, G], BF16)
                mqn = sbt(st, "mqn", [128, 3, G], BF16)
                mrs = sbt(st, "mrs", [128, G], F32)
                rv = sbt(st, "rv", [128, 4, 512], BF16)
                rgs = sbt(st, "rgs", [128, 4, 512], F32)
                fa = sbt(st, "fa", [4, G], F32)
                Fg = sbt(st, "Fg", [4, G], F32)
                fbb = sbt(st, "fbb", [4, G], F32)
                fcar = sbt(st, "fcar", [4, 1], F32)
                f3 = [sbt(st, f"f3_{i}", [4, G], BF16) for i in range(3)]
                S32A = sbt(st, "S32A", [64, 512], F32)
                SbfA = sbt(st, "SbfA", [64, 512], BF16)
                dcT = sbt(st, "dcT", [64, 512], F32)
                for h in range(4):
                    dc = float(np.exp(128.0 * np.log1p(-2.0 ** (-5.0 - h))))
                    S.op("dve", lambda e: e.memset(dcT[:, h * 128:(h + 1) * 128], dc), writes=[dcT])
                PTa = [sbt(st, f"PTa{i}", [128, 512], BF16) for i in range(2)]
                qda = [sbt(st, f"qda{i}", [64, 4, 128], BF16) for i in range(2)]
                Kda = [sbt(st, f"Kda{i}", [128, 4, 64], BF16) for i in range(2)]
                osq = sbt(st, "osq", [128, 512], F32)
                onrm = sbt(st, "onrm", [128, 512], F32)
                yab = sbt(st, "yab", [128, 512], BF16)
                gst = sbt(st, "gst", [128, 6, 4], F32)
                yaTg = sbt(st, "yaTg", [128, 4, G], BF16)
                rot = [0]

                def rope_evac(ps_a, ps_b, dst, rows=64):
                    i = rot[0] % 2
                    rot[0] += 1
                    S.op("dve", lambda e: e.tensor_tensor(t1[i][0:rows, :], ps_a[0:rows, :], cosT[0:rows, :], ALU.mult), reads=[ps_a, cosT], writes=[t1[i]])
                    S.op("dve", lambda e: e.tensor_tensor(t2[i][0:rows, :], ps_b[0:rows, :], sinT[0:rows, :], ALU.mult), reads=[ps_b, sinT], writes=[t2[i]])
                    S.op("pool", lambda e: e.tensor_tensor(dst[0:rows, :], t1[i][0:rows, :], t2[i][0:rows, :], ALU.add), reads=[t1[i], t2[i]], writes=[dst])

                def projA(c0, m, rhsbuf=None, nk=KC, wt=None, rsel=None):
                    ps = nps()
                    wt_ = WA if wt is None else wt
                    rb = hT if rhsbuf is None else rhsbuf
                    for kc in range(nk):
                        S.op("pe", lambda e: e.matmul(ps[0:m, :], wt_[:, kc, c0:c0 + m], rb[:, kc, :], start=(kc == 0), stop=(kc == nk - 1)),
                             reads=[wt_, rb], writes=[ps])
                    return ps

                sg = [0]

                def stage_out(ps, m, dst_ap, scale=None):
                    b = stg[sg[0] % 3]
                    sg[0] += 1
                    S.op("act", lambda e: e.activation(b[0:m, :], ps[0:m, :], AF.Copy), reads=[ps], writes=[b])
                    S.dma("sp", dst_ap, b[0:m, :], reads=[b])

                for g in range(NG):
                    s = g // (NG // NS)
                    g0 = g * G
                    first = (g % (NG // NS) == 0)
                    v = vec[l][s]
                    for half in range(2):
                        S.dma("sp" if half == 0 else "act", x32[:, half * 4:(half + 1) * 4, :],
                              xsrc[half * 4:(half + 1) * 4, :, g0:g0 + G].rearrange("k p t -> p k t"), writes=[x32])
                    for ci in range(g * 3, (g + 1) * 3):
                        if ci < 32:
                            rs = slice(ci * 128, (ci + 1) * 128)
                            S.dma("pool", Wcb[rs, 0:8192].rearrange("r (j c) -> r j c", c=2048),
                                  Wl["W13"][ci * 512:(ci + 1) * 512, :].rearrange("(r j) c -> r j c", j=4))
                        else:
                            c2 = ci - 32
                            rs = slice(c2 * 256, (c2 + 1) * 256)
                            S.dma("pool", Wcb[rs, 8192:12288].rearrange("r (j c) -> r j c", c=2048),
                                  Wl["W2"][c2 * 512:(c2 + 1) * 512, :].rearrange("(r j) c -> r j c", j=2))
                    norm_mod(x32, G, sq, rstd, tmp, lambda kc: v[:, 0, kc:kc + 1], lambda kc: v[:, 1, kc:kc + 1], out_bf=hT)
                    S.dma("act", hTd[:, :, g0:g0 + G].rearrange("k p t -> p k t"), hT[:], reads=[hT])
                    if cut == 2:
                        break
                    S.dma("sp", posi[:], pos_in[0:1, g0:g0 + G].partition_broadcast(128), writes=[posi])
                    S.op("dve", lambda e: e.tensor_copy(ang[:], posi[:]), reads=[posi], writes=[ang])
                    S.op("dve", lambda e: e.tensor_scalar(ang[:], ang[:], cst[:, C_INVF:C_INVF + 1], None, ALU.mult), reads=[ang, cst], writes=[ang])
                    for which, dstT in ((0, sinT), (1, cosT)):
                        shift = 0.0 if which == 0 else math.pi / 2
                        S.op("dve", lambda e: e.tensor_scalar(rr[:], ang[:], shift, 1.0 / (2 * math.pi), ALU.add, ALU.mult), reads=[ang], writes=[rr])
                        S.op("dve", lambda e: e.tensor_copy(ki[:], rr[:]), reads=[rr], writes=[ki])
                        S.op("dve", lambda e: e.tensor_copy(kf[:], ki[:]), reads=[ki], writes=[kf])
                        S.op("dve", lambda e: e.tensor_scalar(rr[:], ang[:], shift, None, ALU.add), reads=[ang], writes=[rr])
                        S.op("dve", lambda e: e.scalar_tensor_tensor(rr[:], kf[:], -6.28125, rr[:], ALU.mult, ALU.add), reads=[kf, rr], writes=[rr])
                        S.op("dve", lambda e: e.scalar_tensor_tensor(rr[:], kf[:], -(2 * math.pi - 6.28125), rr[:], ALU.mult, ALU.add), reads=[kf, rr], writes=[rr])
                        S.op("dve", lambda e: e.tensor_scalar(msk[:], rr[:], math.pi, -2 * math.pi, ALU.is_gt, ALU.mult), reads=[rr], writes=[msk])
                        S.op("dve", lambda e: e.tensor_tensor(rr[:], rr[:], msk[:], ALU.add), reads=[rr, msk], writes=[rr])
                        S.op("dve", lambda e: e.tensor_scalar(msk[:], rr[:], -math.pi, 2 * math.pi, ALU.is_lt, ALU.mult), reads=[rr], writes=[msk])
                        S.op("dve", lambda e: e.tensor_tensor(rr[:], rr[:], msk[:], ALU.add), reads=[rr, msk], writes=[rr])
                        S.op("dve", lambda e: e.tensor_scalar(rr[:], rr[:], math.pi, -math.pi, ALU.min, ALU.max), reads=[rr], writes=[rr])
                        if which == 0:
                            S.op("act", lambda e: e.activation(dstT[:], rr[:], AF.Sin, scale=cst[:, C_SGN:C_SGN + 1]), reads=[rr, cst], writes=[dstT])
                        else:
                            S.op("act", lambda e: e.activation(dstT[:], rr[:], AF.Sin), reads=[rr], writes=[dstT])
                    if cut == 3:
                        break
                    for p in range(2):
                        pa = projA(0 + p * 128, 128)
                        pb = projA(256 + p * 128, 128)
                        rope_evac(pa, pb, R2q[p], rows=128)
                        S.dma("sp", rqO[p][:], R2q[p][64:128, :], reads=[R2q[p]], writes=[rqO[p]])
                        pa = projA(512 + p * 128, 128)
                        pb = projA(768 + p * 128, 128)
                        rope_evac(pa, pb, R2k[p], rows=128)
                        S.dma("act", rkO[p][:], R2k[p][64:128, :], reads=[R2k[p]], writes=[rkO[p]])
                    if cut == 4:
                        break
                    for h in range(4):
                        stage_out(projA(1024 + h * 128, 128), 128, fqT[h, :, g0:g0 + G])
                        stage_out(projA(1536 + h * 128, 128), 128, fkT[h, :, g0:g0 + G])
                    if cut == 5:
                        break
                    for j in range(3):
                        ps = projA(2048 + j * 128, 128)
                        S.op("act", lambda e: e.activation(mq32[:, j, :], ps[:], AF.Copy), reads=[ps], writes=[mq32])
                    S.op("act", lambda e: e.activation(msq[:], mq32[:], AF.Square), reads=[mq32], writes=[msq])
                    ssq = nps()
                    for j in range(2):
                        S.op("pe", lambda e: e.matmul(ssq[:], onesb[:], msq[:, j, :], start=(j == 0), stop=(j == 1)), reads=[onesb, msq], writes=[ssq])
                    S.op("act", lambda e: e.activation(mrs[:], ssq[:], AF.Sqrt, bias=epsc[:], scale=1.0 / 256), reads=[ssq, epsc], writes=[mrs])
                    S.op("dve", lambda e: e.reciprocal(mrs[:], mrs[:]), reads=[mrs], writes=[mrs])
                    for j in range(2):
                        S.op("dve", lambda e: e.scalar_tensor_tensor(mqn[:, j, :], mq32[:, j, :], gq[:, j:j + 1], mrs[:], ALU.mult, ALU.mult), reads=[mq32, gq, mrs], writes=[mqn])
                    ssk = nps()
                    S.op("pe", lambda e: e.matmul(ssk[:], onesb[:], msq[:, 2, :], start=True, stop=True), reads=[onesb, msq], writes=[ssk])
                    S.op("act", lambda e: e.activation(mrs[:], ssk[:], AF.Sqrt, bias=epsc[:], scale=1.0 / 128), reads=[ssk, epsc], writes=[mrs])
                    S.op("dve", lambda e: e.reciprocal(mrs[:], mrs[:]), reads=[mrs], writes=[mrs])
                    S.op("dve", lambda e: e.scalar_tensor_tensor(mqn[:, 2, :], mq32[:, 2, :], gkv[:, 0:1], mrs[:], ALU.mult, ALU.mult), reads=[mq32, gkv, mrs], writes=[mqn])
                    pa = projA(2432, 64)
                    pb = projA(2496, 64)
                    bb = stg[sg[0] % 3]
                    sg[0] += 1
                    rope_evac(pa, pb, bb)
                    S.dma("sp", kpeT[:, g0:g0 + G], bb[0:64, :], reads=[bb])
                    for h in range(4):
                        stage_out(projA(h * 128, 128, rhsbuf=mqn, nk=2, wt=wq), 128, mqnT[h, :, g0:g0 + G])
                        pa = projA(512 + h * 64, 64, rhsbuf=mqn, nk=2, wt=wq)
                        pb = projA(768 + h * 64, 64, rhsbuf=mqn, nk=2, wt=wq)
                        bb = stg[sg[0] % 3]
                        sg[0] += 1
                        rope_evac(pa, pb, bb)
                        S.dma("sp", mqpT[h, :, g0:g0 + G], bb[0:64, :], reads=[bb])
                        ps = nps()
                        S.op("pe", lambda e: e.matmul(ps[:], wkv[:, h * 128:(h + 1) * 128], mqn[:, 2, :], start=True, stop=True), reads=[wkv, mqn], writes=[ps])
                        stage_out(ps, 128, mknT[h, :, g0:g0 + G])
                    if cut == 6:
                        break
                    ps = projA(2560, 4)
                    S.op("act", lambda e: e.activation(fa[:], ps[0:4, :], AF.Exp, bias=nfb[:], scale=-1.0), reads=[ps, nfb], writes=[fa])
                    S.op("act", lambda e: e.activation(fa[:], fa[:], AF.Ln, bias=onec[0:4, :], scale=1.0), reads=[fa, onec], writes=[fa])
                    if first:
                        S.op("dve", lambda e: e.memset(fcar[:], 0.0), writes=[fcar])
                    S.op("dve", lambda e: e.tensor_tensor_scan(Fg[:], ones512[:], fa[:], fcar[:], ALU.mult, ALU.subtract), reads=[ones512, fa, fcar], writes=[Fg])
                    S.op("dve", lambda e: e.tensor_copy(fcar[:], Fg[:, G - 1:G]), reads=[Fg], writes=[fcar])
                    S.dma("sp", F32d[:, g0:g0 + G], Fg[:], reads=[Fg])
                    S.op("dve", lambda e: e.tensor_scalar(fbb[:], Fg[:], 1.0 / SC_FOX, None, ALU.mult), reads=[Fg], writes=[fbb])
                    for i in range(3):
                        S.op("dve", lambda e: e.tensor_copy(f3[i][:], fbb[:]), reads=[fbb], writes=[f3[i]])
                        if i < 2:
                            S.op("dve", lambda e: e.tensor_tensor(fbb[:], fbb[:], f3[i][:], ALU.subtract), reads=[fbb, f3[i]], writes=[fbb])
                        S.dma("sp", F3d[:, i, g0:g0 + G], f3[i][:], reads=[f3[i]])
                    if cut == 7:
                        break
                    for tt in range(4):
                        tsl = slice(tt * 128, (tt + 1) * 128)
                        for j in range(3):
                            ps = nps()
                            for kc in range(KC):
                                S.op("pe", lambda e: e.matmul(ps[:], hT[:, kc, tsl], WB[:, kc, j * 512:(j + 1) * 512], start=(kc == 0), stop=(kc == KC - 1)),
                                     reads=[hT, WB], writes=[ps])
                            if j == 0:
                                S.op("act", lambda e: e.activation(rv[:, tt, :], ps[:], AF.Copy), reads=[ps], writes=[rv])
                            elif j == 1:
                                S.op("act", lambda e: e.activation(rgs[:, tt, :], ps[:], AF.Silu), reads=[ps], writes=[rgs])
                            else:
                                stage_out(ps, 128, fvd[g0 + tt * 128:g0 + (tt + 1) * 128, :])
                        ps = nps()
                        S.op("pe", lambda e: e.matmul(ps[:], mqn[:, 2, tsl], wkv[:, 512:1024], start=True, stop=True), reads=[mqn, wkv], writes=[ps])
                        stage_out(ps, 128, mvd[g0 + tt * 128:g0 + (tt + 1) * 128, :])
                    if cut == 8:
                        break
                    if first:
                        S.op("dve", lambda e: e.memset(S32A[:], 0.0), writes=[S32A])
                        S.op("dve", lambda e: e.memset(SbfA[:], 0.0), writes=[SbfA])
                    pend_norm = [None]
                    for tt in (range(4) if os.environ.get("KSKIPRET") is None else []):
                        tsl = slice(tt * 128, (tt + 1) * 128)
                        pso = nps(6, 8)
                        u2 = tt % 2
                        pss = nps()
                        psk = nps()
                        pskb = psk[:].bitcast(BF16)
                        for h in range(4):
                            S.op("pe", lambda e: e.matmul(pss[:, h * 128:(h + 1) * 128], rkA(h, tsl), rqA(h, tsl), start=True, stop=True), reads=[rkB[h], rqB[h]], writes=[pss])
                        for h in range(4):
                            S.op("pe", lambda e: e.transpose(pskb[:, h * 64:(h + 1) * 64], rkA(h, tsl), identb[0:64, 0:64]), reads=[rkB[h], identb], writes=[psk])
                        S.op("dve", lambda e: e.tensor_tensor(PTa[u2][:], pss[:], cst[:, C_DM:C_DM + 512], ALU.mult), reads=[pss, cst], writes=[PTa[u2]])
                        S.op("dve", lambda e: e.tensor_tensor(Kda[u2][:], pskb[:, 0:256].rearrange("p (h d) -> p h d", h=4),
                                                              cst[:, C_DO:C_DO + 4].unsqueeze(2).to_broadcast([128, 4, 64]), ALU.mult), reads=[psk, cst], writes=[Kda[u2]])
                        for h in range(4):
                            S.op("pool", lambda e: e.tensor_tensor(qda[u2][:, h, :], rqA(h, tsl), cst[0:64, C_DI + h * 128:C_DI + (h + 1) * 128], ALU.mult),
                                 reads=[rqB[h], cst], writes=[qda[u2]])
                        for h in range(4):
                            hs = slice(h * 128, (h + 1) * 128)
                            S.op("pe", lambda e: e.matmul(pso[:, hs], PTa[u2][:, hs], rv[:, tt, hs], start=True, stop=False), reads=[PTa[u2], rv], writes=[pso])
                            S.op("pe", lambda e: e.matmul(pso[:, hs], qda[u2][:, h, :], SbfA[:, hs], start=False, stop=True), reads=[qda[u2], SbfA], writes=[pso])
                        pkv = nps()
                        for h in range(4):
                            hs = slice(h * 128, (h + 1) * 128)
                            S.op("pe", lambda e: e.matmul(pkv[0:64, hs], Kda[u2][:, h, :], rv[:, tt, hs], start=True, stop=True), reads=[Kda[u2], rv], writes=[pkv])
                        S.op("dve", lambda e: e.tensor_tensor(S32A[:], S32A[:], dcT[:], ALU.mult), reads=[S32A, dcT], writes=[S32A])
                        S.op("dve", lambda e: e.tensor_tensor(S32A[:], S32A[:], pkv[0:64, :], ALU.add), reads=[S32A, pkv], writes=[S32A])
                        S.op("pool", lambda e: e.tensor_copy(SbfA[:], S32A[:]), reads=[S32A], writes=[SbfA])
                        def ret_norm(pso=pso, tt=tt, tsl=tsl):
                            pv = pso[:].rearrange("p (h e) -> p h e", h=4)
                            S.op("dve", lambda e: e.tensor_reduce(gst[:, 0, :], pv, AX.X, ALU.add), reads=[pso], writes=[gst])
                            S.op("act", lambda e: e.activation(osq[:], pso[:], AF.Square), reads=[pso], writes=[osq])
                            S.op("dve", lambda e: e.tensor_reduce(gst[:, 1, :], osq[:].rearrange("p (h e) -> p h e", h=4), AX.X, ALU.add), reads=[osq], writes=[gst])
                            S.op("dve", lambda e: e.tensor_scalar(gst[:, 2, :], gst[:, 0, :], 1.0 / 128, None, ALU.mult), reads=[gst], writes=[gst])
                            S.op("dve", lambda e: e.tensor_tensor(gst[:, 3, :], gst[:, 2, :], gst[:, 2, :], ALU.mult), reads=[gst], writes=[gst])
                            S.op("dve", lambda e: e.scalar_tensor_tensor(gst[:, 3, :], gst[:, 1, :], 1.0 / 128, gst[:, 3, :], ALU.mult, ALU.subtract), reads=[gst], writes=[gst])
                            S.op("act", lambda e: e.activation(gst[:, 4, :], gst[:, 3, :], AF.Sqrt, bias=epsc[:], scale=1.0), reads=[gst, epsc], writes=[gst])
                            S.op("dve", lambda e: e.reciprocal(gst[:, 4, :], gst[:, 4, :]), reads=[gst], writes=[gst])
                            S.op("dve", lambda e: e.scalar_tensor_tensor(gst[:, 5, :], gst[:, 2, :], -1.0, gst[:, 4, :], ALU.mult, ALU.mult), reads=[gst], writes=[gst])
                            for h in range(4):
                                hs = slice(h * 128, (h + 1) * 128)
                                S.op("act", lambda e: e.activation(onrm[:, hs], pso[:, hs], AF.Identity, bias=gst[:, 5, h:h + 1], scale=gst[:, 4, h:h + 1]), reads=[pso, gst], writes=[onrm])
                            S.op("pool", lambda e: e.tensor_tensor(yab[:], onrm[:], rgs[:, tt, :], ALU.mult), reads=[onrm, rgs], writes=[yab])
                            pst = nps()
                            pstb = pst[:].bitcast(BF16)
                            for kc in range(4):
                                S.op("pe", lambda e: e.transpose(pstb[:, kc * 128:(kc + 1) * 128], yab[:, kc * 128:(kc + 1) * 128], identb[:]), reads=[yab, identb], writes=[pst])
                            S.op("act", lambda e: e.activation(yaTg[:, :, tsl], pstb[:, 0:512].rearrange("p (k t) -> p k t", k=4), AF.Copy), reads=[pst], writes=[yaTg])

                        if pend_norm[0] is not None:
                            pend_norm[0]()
                        pend_norm[0] = ret_norm
                    if pend_norm[0] is not None:
                        pend_norm[0]()
                    pend_norm[0] = None
                    if cut in (9, 10, 11):
                        break
                    S.dma("act", yTd[0, :, :, g0:g0 + G].rearrange("k p t -> p k t"), yaTg[:], reads=[yaTg])
                S.barrier()
            if stop_after == "A":
                lst.close()
                break

            psrange[:] = [0, 8]
            cwst = contextlib.ExitStack()
            cwst.__enter__()
            Wg = wload(cwst, "Wg", [128, KC, 3 * D], Wl["Wg"], defer=True)
            Wbr = wload(cwst, "Wbr", [128, 12, D], Wl["Wbr"], defer=True)
            Wo = wload(cwst, "Wo", [128, KC, D], Wl["Wo"], defer=True)
            with contextlib.ExitStack() as st:
                NB = 2
                QT = [sbt(st, f"QT{i}", [128, SEQ], BF16) for i in range(NB)]
                KT = [sbt(st, f"KT{i}", [128, SEQ], BF16) for i in range(NB)]
                VV = [sbt(st, f"VV{i}", [128, 32, 128], BF16) for i in range(NB)]
                XQ = [sbt(st, f"XQ{i}", [64, SEQ], BF16) for i in range(NB)]
                XK = [sbt(st, f"XK{i}", [64, SEQ], BF16) for i in range(NB)]
                nFk = [sbt(st, f"nFk{i}", [128, 32], F32) for i in range(NB)]
                yTs = [sbt(st, f"yTs{i}", [128, SEQ], BF16) for i in range(NB)]
                PT = [sbt(st, f"PT{i}", [128, 512], BF16) for i in range(4)]
                rec = sbt(st, "rec", [128, 512], F32)
                zero_c = sbt(st, "zero_c", [128, 1], F32)
                S.op("dve", lambda e: e.memset(zero_c[:], 0.0), writes=[zero_c])
                SP = [PS[0], PS[1], PS[2], PS[3]]
                OP = [PS[4], PS[5]]
                RP = [PS[6], PS[7]]
                unit = 0
                for kind in ("fox", "mla"):
                    for s in range(NS):
                        for h in range(4):
                            u = unit % NB
                            unit += 1
                            s0 = s * SEQ
                            if kind == "fox":
                                S.dma("sp", QT[u][:], fqT[h, :, s0:s0 + SEQ], writes=[QT[u]])
                                S.dma("act", KT[u][:], fkT[h, :, s0:s0 + SEQ], writes=[KT[u]])
                                S.dma("sp", VV[u][:], fvd[s0:s0 + SEQ, h * 128:(h + 1) * 128].rearrange("(b p) e -> p b e", p=128), writes=[VV[u]])
                                S.dma("act", XQ[u][0:3, :], F3d[h, :, s0:s0 + SEQ], writes=[XQ[u]])
                                with nc.allow_non_contiguous_dma(reason="small forget-bias transpose load"):
                                    S.dma("sp", nFk[u][:], F32d[h, s0:s0 + SEQ].rearrange("(b p) -> p b", p=128), writes=[nFk[u]])
                                S.op("dve", lambda e: e.tensor_scalar(nFk[u][:], nFk[u][:], -1.0, None, ALU.mult), reads=[nFk[u]], writes=[nFk[u]])
                                sc = SC_FOX
                                ybr = 1
                            else:
                                S.dma("sp", QT[u][:], mqnT[h, :, s0:s0 + SEQ], writes=[QT[u]])
                                S.dma("act", KT[u][:], mknT[h, :, s0:s0 + SEQ], writes=[KT[u]])
                                S.dma("sp", VV[u][:], mvd[s0:s0 + SEQ, h * 128:(h + 1) * 128].rearrange("(b p) e -> p b e", p=128), writes=[VV[u]])
                                S.dma("act", XQ[u][:], mqpT[h, :, s0:s0 + SEQ], writes=[XQ[u]])
                                S.dma("sp", XK[u][:], kpeT[:, s0:s0 + SEQ], writes=[XK[u]])
                                sc = SC_MLA
                                ybr = 2
                            pairs = [(g, kb) for g in range(8) for kb in range(4 * g + 4)]

                            def emit_S(idx):
                                g, kb = pairs[idx]
                                j = kb - 4 * g
                                c0 = max(j, 0) * 128
                                sp_ = SP[idx % 4]
                                q0 = g * 512 + c0
                                q1 = (g + 1) * 512
                                ksl = slice(kb * 128, (kb + 1) * 128)
                                S.op("pe", lambda e: e.matmul(sp_[:, c0:512], KT[u][:, ksl], QT[u][:, q0:q1], start=True, stop=False), reads=[KT[u], QT[u]], writes=[sp_])
                                last2 = (j < 0)
                                if kind == "fox":
                                    S.op("pe", lambda e: e.matmul(sp_[:, c0:512], onesb[0:3, :], XQ[u][0:3, q0:q1], start=False, stop=last2), reads=[onesb, XQ[u]], writes=[sp_])
                                else:
                                    S.op("pe", lambda e: e.matmul(sp_[:, c0:512], XK[u][:, ksl], XQ[u][:, q0:q1], start=False, stop=last2), reads=[XK[u], XQ[u]], writes=[sp_])
                                if j >= 0:
                                    S.op("pe", lambda e: e.matmul(sp_[:, c0:c0 + 128], identb[:], negtri[:], start=False, stop=True), reads=[identb, negtri], writes=[sp_])

                            def emit_P(idx):
                                g, kb = pairs[idx]
                                j = kb - 4 * g
                                c0 = max(j, 0) * 128
                                sp_ = SP[idx % 4]
                                pt = PT[idx % 4]
                                bias = nFk[u][:, kb:kb + 1] if kind == "fox" else zero_c[:]
                                S.op("act", lambda e: e.activation(pt[:, c0:512], sp_[:, c0:512], AF.Exp, bias=bias, scale=sc), reads=[sp_, nFk[u], zero_c], writes=[pt])
                                op_ = OP[g % 2]
                                rp_ = RP[g % 2]
                                lastk = 4 * g + 3
                                S.op("pe", lambda e: e.matmul(op_[:, c0:512], VV[u][:, kb, :], pt[:, c0:512], start=(kb == 0), stop=(kb == lastk)), reads=[VV[u], pt], writes=[op_])
                                S.op("pe", lambda e: e.matmul(rp_[:, c0:512], onesb[:], pt[:, c0:512], start=(kb == 0), stop=(kb == lastk)), reads=[onesb, pt], writes=[rp_])
                                if kb == lastk:
                                    S.op("dve", lambda e: e.reciprocal(rec[:], rp_[:]), reads=[rp_], writes=[rec])
                                    S.op("dve", lambda e: e.tensor_tensor(yTs[u][:, g * 512:(g + 1) * 512], op_[:], rec[:], ALU.mult), reads=[op_, rec], writes=[yTs[u]])

                            emit_S(0)
                            emit_S(1)
                            for idx in range(len(pairs)):
                                if idx + 2 < len(pairs):
                                    emit_S(idx + 2)
                                emit_P(idx)
                            S.dma("sp", yTd[ybr, h, :, s0:s0 + SEQ], yTs[u][:], reads=[yTs[u]])
                            if unit == 3:
                                wfill(Wg, [128, KC, 3 * D], Wl["Wg"], after=[yTs[u]])
                                wfill(Wbr, [128, 12, D], Wl["Wbr"], after=[yTs[u]])
                                wfill(Wo, [128, KC, D], Wl["Wo"], after=[yTs[u]])
                S.barrier()
            if stop_after == "B":
                cwst.close()
                lst.close()
                break

            ohst = contextlib.ExitStack()
            ohst.__enter__()
            OH1 = sbt(ohst, f"OH1_{l}", [128, NE, NT], F32)
            OH2 = sbt(ohst, f"OH2_{l}", [128, NE, NT], F32)
            lgA = sbt(ohst, f"lgA_{l}", [128, NT, 36], F32)
            with contextlib.ExitStack() as st:
                Wr = sbt(st, "Wr", [128, KC, 36], F32)
                brt = sbt(st, "brt", [128, 36], F32)
                S.dma("sp", Wr[:], Wl["Wr"], writes=[Wr])
                S.dma("sp", brt[:], Wl["br"], writes=[brt])
                h2f = sbt(st, "h2f", [128, KC, G], F32)
                htk = [sbt(st, f"htk{i}", [128, D], BF16) for i in range(2)]
                gb = sbt(st, "gb", [128, 24], F32)
                S.dma("sp", gb[:], Wl["gb"], writes=[gb])
                x32 = sbt(st, "x32c", [128, KC, G], F32)
                hT = sbt(st, "hTc", [128, KC, G], BF16)
                yT = sbt(st, "yTc", [128, 12, G], BF16)
                mT = sbt(st, "mTc", [128, KC, G], BF16)
                gate = [sbt(st, f"gate{i}", [128, G], F32) for i in range(3)]
                prod = [sbt(st, f"prod{i}", [128, G], F32) for i in range(3)]
                macc = sbt(st, "macc", [128, G], F32)
                for g in range(NG):
                    s = g // (NG // NS)
                    g0 = g * G
                    v = vec[l][s]
                    S.dma("sp", hT[:], hTd[:, :, g0:g0 + G].rearrange("k p t -> p k t"), writes=[hT])
                    for i in range(3):
                        S.dma("act", yT[:, i * 4:(i + 1) * 4, :], yTd[i, :, :, g0:g0 + G].rearrange("k p t -> p k t"), writes=[yT])
                    for half in range(2):
                        S.dma("sp" if half == 0 else "act", x32[:, half * 4:(half + 1) * 4, :],
                              xsrc[half * 4:(half + 1) * 4, :, g0:g0 + G].rearrange("k p t -> p k t"), writes=[x32])
                    for oc in range(KC):
                        for i in range(3):
                            pg = nps()
                            col = (i * 8 + oc) * 128
                            for kc in range(KC):
                                S.op("pe", lambda e: e.matmul(pg[:], Wg[:, kc, col:col + 128], hT[:, kc, :], start=(kc == 0), stop=(kc == KC - 1)), reads=[Wg, hT], writes=[pg])
                            S.op("act", lambda e: e.activation(gate[i][:], pg[:], AF.Sigmoid, bias=gb[:, i * 8 + oc:i * 8 + oc + 1], scale=1.0), reads=[pg, gb], writes=[gate[i]])
                            pb = nps()
                            for kc in range(4):
                                S.op("pe", lambda e: e.matmul(pb[:], Wbr[:, i * 4 + kc, oc * 128:(oc + 1) * 128], yT[:, i * 4 + kc, :], start=(kc == 0), stop=(kc == 3)), reads=[Wbr, yT], writes=[pb])
                            S.op("dve", lambda e: e.tensor_tensor(prod[i][:], pb[:], gate[i][:], ALU.mult), reads=[pb, gate[i]], writes=[prod[i]])
                        S.op("pool", lambda e: e.tensor_tensor(macc[:], prod[0][:], prod[1][:], ALU.add), reads=[prod[0], prod[1]], writes=[macc])
                        S.op("pool", lambda e: e.tensor_tensor(mT[:, oc, :], macc[:], prod[2][:], ALU.add), reads=[macc, prod[2]], writes=[mT])
                    for oc in range(KC):
                        po = nps()
                        for kc in range(KC):
                            S.op("pe", lambda e: e.matmul(po[:], Wo[:, kc, oc * 128:(oc + 1) * 128], mT[:, kc, :], start=(kc == 0), stop=(kc == KC - 1)), reads=[Wo, mT], writes=[po])
                        S.op("dve", lambda e: e.scalar_tensor_tensor(x32[:, oc, :], po[:], v[:, 2, oc:oc + 1], x32[:, oc, :], ALU.mult, ALU.add), reads=[po, x32], writes=[x32])
                    for half in range(2):
                        S.dma("sp" if half == 0 else "act", xw[half * 4:(half + 1) * 4, :, g0:g0 + G].rearrange("k p t -> p k t"),
                              x32[:, half * 4:(half + 1) * 4, :], reads=[x32])
                    norm_mod(x32, G, mT, prod[0], macc, lambda kc: v[:, 3, kc:kc + 1], lambda kc: v[:, 4, kc:kc + 1], out_f32=h2f)
                    for tt in range(4):
                        jt = g * 4 + tt
                        tsl = slice(tt * 128, (tt + 1) * 128)
                        hb = htk[jt % 2]
                        for half in range(2):
                            pst = nps()
                            for k4 in range(4):
                                kc = half * 4 + k4
                                S.op("pe", lambda e: e.transpose(pst[:, k4 * 128:(k4 + 1) * 128], h2f[:, kc, tsl], identf_ap), reads=[h2f, cst], writes=[pst])
                            if half == 0:
                                S.op("act", lambda e: e.activation(hb[:, 0:512], pst[:], AF.Copy), reads=[pst], writes=[hb])
                            else:
                                S.op("pool", lambda e: e.tensor_copy(hb[:, 512:1024], hb[:, 512:1024]), reads=[hb], writes=[hb]) if False else \
                                    S.op("act", lambda e: e.activation(hb[:, 512:1024], pst[:], AF.Copy), reads=[pst], writes=[hb])
                        S.dma("sp", h2tok[jt * 128:(jt + 1) * 128, :], hb[:], reads=[hb])
                        pl = nps()
                        for kc in range(KC):
                            S.op("pe", lambda e: e.matmul(pl[:, 0:36], h2f[:, kc, tsl], Wr[:, kc, :], start=(kc == 0), stop=(kc == KC - 1)), reads=[h2f, Wr], writes=[pl])
                        S.op("dve", lambda e: e.tensor_tensor(lgA[:, jt, :], pl[:, 0:36], brt[:], ALU.add), reads=[pl, brt], writes=[lgA])
                S.barrier()
            if stop_after == "C1":
                ohst.close()
                cwst.close()
                lst.close()
                break

            with contextlib.ExitStack() as st:
                r1 = sbt(st, "r1", [128, 5, NT], F32)
                ohgA = sbt(st, "ohgA", [128, NT, 4], F32)
                ex4A = sbt(st, "ex4A", [128, NT, 4], F32)
                ingA = sbt(st, "ingA", [128, NT, 8], F32)
                tm8 = sbt(st, "tm8", [128, NT, 8], F32)
                oh1A = sbt(st, "oh1A", [128, NT, 8], F32)
                oh2A = sbt(st, "oh2A", [128, NT, 8], F32)
                def bc(ap, shape):
                    return ap.to_broadcast(shape)
                g4 = lgA[:, :, 0:4]
                S.op("dve", lambda e: e.tensor_reduce(r1[:, 0, :], g4, AX.X, ALU.max), reads=[lgA], writes=[r1])
                S.op("dve", lambda e: e.tensor_tensor(ohgA[:], g4, bc(r1[:, 0, :].unsqueeze(2), [128, NT, 4]), ALU.is_equal), reads=[lgA, r1], writes=[ohgA])
                S.op("dve", lambda e: e.tensor_tensor(ex4A[:], g4, bc(r1[:, 0, :].unsqueeze(2), [128, NT, 4]), ALU.subtract), reads=[lgA, r1], writes=[ex4A])
                S.op("act", lambda e: e.activation(ex4A[:], ex4A[:], AF.Exp), reads=[ex4A], writes=[ex4A])
                S.op("dve", lambda e: e.tensor_reduce(r1[:, 1, :], ex4A[:], AX.X, ALU.add), reads=[ex4A], writes=[r1])
                S.op("dve", lambda e: e.reciprocal(r1[:, 1, :], r1[:, 1, :]), reads=[r1], writes=[r1])
                for gi in range(4):
                    dst = ingA if gi == 0 else tm8
                    S.op("dve", lambda e: e.tensor_tensor(dst[:], lgA[:, :, 4 + 8 * gi:12 + 8 * gi], bc(ohgA[:, :, gi:gi + 1], [128, NT, 8]), ALU.mult), reads=[lgA, ohgA], writes=[dst])
                    if gi > 0:
                        S.op("dve", lambda e: e.tensor_tensor(ingA[:], ingA[:], tm8[:], ALU.add), reads=[ingA, tm8], writes=[ingA])
                S.op("dve", lambda e: e.tensor_reduce(r1[:, 2, :], ingA[:], AX.X, ALU.max), reads=[ingA], writes=[r1])
                S.op("dve", lambda e: e.tensor_tensor(oh1A[:], ingA[:], bc(r1[:, 2, :].unsqueeze(2), [128, NT, 8]), ALU.is_equal), reads=[ingA, r1], writes=[oh1A])
                S.op("dve", lambda e: e.scalar_tensor_tensor(tm8[:], oh1A[:], -1e30, ingA[:], ALU.mult, ALU.add), reads=[oh1A, ingA], writes=[tm8])
                S.op("dve", lambda e: e.tensor_reduce(r1[:, 3, :], tm8[:], AX.X, ALU.max), reads=[tm8], writes=[r1])
                S.op("dve", lambda e: e.tensor_tensor(oh2A[:], tm8[:], bc(r1[:, 3, :].unsqueeze(2), [128, NT, 8]), ALU.is_equal), reads=[tm8, r1], writes=[oh2A])
                S.op("dve", lambda e: e.tensor_tensor(r1[:, 4, :], r1[:, 2, :], r1[:, 3, :], ALU.subtract), reads=[r1], writes=[r1])
                S.op("act", lambda e: e.activation(r1[:, 4, :], r1[:, 4, :], AF.Sigmoid), reads=[r1], writes=[r1])
                S.op("dve", lambda e: e.tensor_tensor(Wts[:, :, 0], r1[:, 4, :], r1[:, 1, :], ALU.mult), reads=[r1], writes=[Wts])
                S.op("dve", lambda e: e.tensor_tensor(Wts[:, :, 1], r1[:, 1, :], Wts[:, :, 0], ALU.subtract), reads=[r1, Wts], writes=[Wts])
                for gi in range(4):
                    gb_ = bc(ohgA[:, :, gi].unsqueeze(1), [128, 8, NT])
                    S.op("dve", lambda e: e.tensor_tensor(OH1[:, gi * 8:(gi + 1) * 8, :], oh1A[:].rearrange("p t j -> p j t"), gb_, ALU.mult), reads=[oh1A, ohgA], writes=[OH1])
                    S.op("dve", lambda e: e.tensor_tensor(OH2[:, gi * 8:(gi + 1) * 8, :], oh2A[:].rearrange("p t j -> p j t"), gb_, ALU.mult), reads=[oh2A, ohgA], writes=[OH2])
                S.barrier()

            with contextlib.ExitStack() as st:
                d1 = sbt(st, f"d1_{l}", [128, NT], I32)
                d2 = sbt(st, f"d2_{l}", [128, NT], I32)
                ib = sbt(st, f"ib_{l}", [128, NBLK], I32)
                NC2 = NE * NT
                Mb = sbt(st, "Mb", [128, NC2], BF16)
                Mf = sbt(st, "Mf", [128, NC2], F32)
                rank = sbt(st, "rank", [128, NC2], F32)
                tot = sbt(st, "tot", [128, NC2], F32)
                gin = sbt(st, "gin", [128, NC2], F32)
                onesN = sbt(st, "onesN", [128, NC2], F32)
                cnt = sbt(st, "cnt", [128, NE], F32)
                cnti = sbt(st, "cnti", [128, NE], I32)
                pcn = sbt(st, "pcn", [128, NE], F32)
                pin = sbt(st, "pin", [128, NE], F32)
                pof = sbt(st, "pof", [128, NE], F32)
                ones32 = sbt(st, "ones32", [128, NE], F32)
                be = sbt(st, "be", [128, NBLK], F32)
                bt = sbt(st, "bt", [128, NBLK], F32)
                eq = sbt(st, "eq", [128, NBLK], F32)
                df = sbt(st, "df", [128, NT], F32)
                o1f = OH1[:].rearrange("p e j -> p (e j)")
                o2f = OH2[:].rearrange("p e j -> p (e j)")
                S.op("dve", lambda e: e.memset(onesN[:], 1.0), writes=[onesN])
                S.op("dve", lambda e: e.memset(ones32[:], 1.0), writes=[ones32])
                S.op("dve", lambda e: e.tensor_tensor(Mf[:], o1f, o2f, ALU.add), reads=[OH1, OH2], writes=[Mf])
                S.op("dve", lambda e: e.tensor_copy(Mb[:], Mf[:]), reads=[Mf], writes=[Mb])
                for c in range(NC2 // 512):
                    cs = slice(c * 512, (c + 1) * 512)
                    p1 = nps()
                    S.op("pe", lambda e: e.matmul(p1[:], ustr[:], Mb[:, cs], start=True, stop=True), reads=[ustr, Mb], writes=[p1])
                    S.op("act", lambda e: e.activation(rank[:, cs], p1[:], AF.Copy), reads=[p1], writes=[rank])
                    p2 = nps()
                    S.op("pe", lambda e: e.matmul(p2[:], onesb[:], Mb[:, cs], start=True, stop=True), reads=[onesb, Mb], writes=[p2])
                    S.op("act", lambda e: e.activation(tot[:, cs], p2[:], AF.Copy), reads=[p2], writes=[tot])
                S.op("dve", lambda e: e.tensor_tensor_scan(gin[:], onesN[:], tot[:], 0.0, ALU.mult, ALU.add), reads=[onesN, tot], writes=[gin])
                S.op("dve", lambda e: e.tensor_tensor(gin[:], gin[:], tot[:], ALU.subtract), reads=[gin, tot], writes=[gin])
                S.op("dve", lambda e: e.tensor_reduce(cnt[:], tot[:].rearrange("p (e j) -> p e j", e=NE), AX.X, ALU.add), reads=[tot], writes=[cnt])
                S.op("dve", lambda e: e.tensor_scalar(cnti[:], cnt[:], 127.0, None, ALU.add), reads=[cnt], writes=[cnti])
                S.op("dve", lambda e: e.tensor_single_scalar(cnti[:], cnti[:], 7, ALU.arith_shift_right), reads=[cnti], writes=[cnti])
                S.op("dve", lambda e: e.tensor_copy(pcn[:], cnti[:]), reads=[cnti], writes=[pcn])
                S.op("dve", lambda e: e.tensor_scalar(pcn[:], pcn[:], 128.0, None, ALU.mult), reads=[pcn], writes=[pcn])
                S.op("dve", lambda e: e.tensor_tensor_scan(pin[:], ones32[:], pcn[:], 0.0, ALU.mult, ALU.add), reads=[ones32, pcn], writes=[pin])
                S.op("dve", lambda e: e.tensor_tensor(pof[:], pin[:], pcn[:], ALU.subtract), reads=[pin, pcn], writes=[pof])
                S.op("dve", lambda e: e.tensor_tensor(pof[:], pof[:], gin[:].rearrange("p (e j) -> p e j", e=NE)[:, :, 0], ALU.subtract), reads=[pof, gin], writes=[pof])
                S.op("dve", lambda e: e.tensor_tensor(gin[:], gin[:], rank[:], ALU.add), reads=[gin, rank], writes=[gin])
                for ee in range(NE):
                    S.op("dve", lambda e: e.tensor_scalar(gin[:, ee * NT:(ee + 1) * NT], gin[:, ee * NT:(ee + 1) * NT], pof[:, ee:ee + 1], None, ALU.add), reads=[gin, pof], writes=[gin])
                for k, (oh, dd) in enumerate(((o1f, d1), (o2f, d2))):
                    S.op("dve", lambda e: e.tensor_tensor(Mf[:], oh, gin[:], ALU.mult), reads=[OH1, OH2, gin], writes=[Mf])
                    S.op("dve", lambda e: e.tensor_reduce(df[:], Mf[:].rearrange("p (e j) -> p j e", e=NE), AX.X, ALU.add), reads=[Mf], writes=[df])
                    S.op("dve", lambda e: e.tensor_copy(dd[:], df[:]), reads=[df], writes=[dd])
                S.op("dve", lambda e: e.memset(be[:], 0.0), writes=[be])
                for ee in range(NE):
                    S.op("dve", lambda e: e.scalar_tensor_tensor(be[:], cst[:, C_THR:C_THR + NBLK], pin[:, ee:ee + 1], be[:], ALU.is_ge, ALU.add), reads=[cst, pin, be], writes=[be])
                S.op("dve", lambda e: e.tensor_scalar(be[:], be[:], float(NE - 1), None, ALU.min), reads=[be], writes=[be])
                S.op("dve", lambda e: e.memset(eq[:], 0.0), writes=[eq])
                S.op("dve", lambda e: e.tensor_tensor(eq[:, 3:NBLK], be[:, 3:NBLK], be[:, 0:NBLK - 3], ALU.is_equal), reads=[be], writes=[eq])
                S.op("dve", lambda e: e.tensor_scalar(bt[:], be[:], 128.0, cst[:, C_IOTA:C_IOTA + 1], ALU.mult, ALU.add), reads=[be, cst], writes=[bt])
                S.op("dve", lambda e: e.scalar_tensor_tensor(bt[:], eq[:], SKIP_OFF, bt[:], ALU.mult, ALU.add), reads=[eq, bt], writes=[bt])
                S.op("dve", lambda e: e.tensor_copy(ib[:], bt[:]), reads=[bt], writes=[ib])
                for nm, src, dst in (("d1", d1, d1v), ("d2", d2, d2v), ("ib", ib, ibv)):
                    shp = [128] + [int(x) for x in list(src.ap.shape)[1:]]
                    scr = nc.dram_tensor(f"ix_{nm}_{l}", shp, I32, kind="Internal").ap()
                    S.dma("sp", scr, src[:], reads=[src])
                    S.barrier()
                    S.dma("sp", dst[:], scr, writes=[dst])
                if "rdbg" in dbg:
                    S.op("dve", lambda e: e.tensor_copy(Mf[:, 0:NT], d1[:]), reads=[d1], writes=[Mf])
                    S.op("dve", lambda e: e.tensor_copy(Mf[:, NT:2 * NT], d2[:]), reads=[d2], writes=[Mf])
                    S.op("dve", lambda e: e.tensor_copy(Mf[:, 2 * NT:4 * NT], Wts[:].rearrange("p j k -> p (j k)")), reads=[Wts], writes=[Mf])
                    S.dma("sp", rdbg, Mf[:, 0:4 * NT], reads=[Mf])
                S.barrier()
            ohst.close()
            cwst.close()
            if stop_after == "C2":
                lst.close()
                break

            xs = nc.dram_tensor(f"xs_l{l}", [NSLOT, D], BF16, kind="Internal").ap()
            Ys = nc.dram_tensor(f"Ys_l{l}", [NSLOT, D], F32, kind="Internal").ap()
            with contextlib.ExitStack() as st:
                hb = [sbt(st, f"hbs{i}", [128, D], BF16) for i in range(3)]
                for jt in range(NT):
                    b = hb[jt % 3]
                    S.dma("sp", b[:], h2tok[jt * 128:(jt + 1) * 128, :], writes=[b])
                    for dd in (d1v, d2v):
                        S.dma("pool", None, None, reads=[b, dd], fn=lambda e: e.indirect_dma_start(
                            out=xs, out_offset=bass.IndirectOffsetOnAxis(ap=dd[:, jt:jt + 1], axis=0), in_=b[:], in_offset=None,
                            bounds_check=r_slot, oob_is_err=False))
                S.barrier()

            if stop_after == "D1":
                lst.close()
                break
            with contextlib.ExitStack() as st:
                wcb = [sbt(st, f"wcb_{i}", [128, 12288], BF16) for i in range(3)]
                xb = [sbt(st, f"xb{i}", [128, D], BF16) for i in range(3)]
                xbT = [sbt(st, f"xbT{i}", [128, KC, 128], BF16) for i in range(2)]
                sil = [sbt(st, f"sil{i}", [128, 512], F32) for i in range(2)]
                hid = [sbt(st, f"hid{i}", [128, 512], BF16) for i in range(2)]
                hidT = [sbt(st, f"hidT{i}", [128, 4, 128], BF16) for i in range(2)]
                ysb = [sbt(st, f"ysb{i}", [128, D], F32) for i in range(2)]

                def d2_s1a(b):
                    u = b % 2
                    w = b % 3
                    S.dma("pool", None, None, reads=[ibv], writes=[wcb[w]], fn=lambda e: e.indirect_dma_start(
                        out=wcb[w][:], out_offset=None, in_=Wcb, in_offset=bass.IndirectOffsetOnAxis(ap=ibv[:, b:b + 1], axis=0),
                        bounds_check=r_e, oob_is_err=False))
                    xbb = xb[b % 3]
                    S.dma("sp", xbb[:], xs[b * 128:(b + 1) * 128, :], writes=[xbb])
                    pst = PS[u]
                    pstb = pst[:].bitcast(BF16)
                    for kc in range(KC):
                        S.op("pe", lambda e: e.transpose(pstb[:, kc * 128:(kc + 1) * 128], xbb[:, kc * 128:(kc + 1) * 128], identb[:]), reads=[xbb, identb], writes=[pst])
                    xv = xbT[u][:].rearrange("p k t -> p (k t)")
                    S.op("act", lambda e: e.activation(xv[:, 0:512], pstb[:, 0:512], AF.Copy), reads=[pst], writes=[xbT[u]])
                    S.op("dve", lambda e: e.tensor_copy(xv[:, 512:1024], pstb[:, 512:1024]), reads=[pst], writes=[xbT[u]])

                def d2_s1b(b):
                    u = b % 2
                    w = b % 3
                    w13v = wcb[w][:, 0:8192].rearrange("p (a c) -> p a c", c=512)
                    p1 = PS[3]
                    p3 = PS[4]
                    for kc in range(KC):
                        S.op("pe", lambda e: e.matmul(p1[:], xbT[u][:, kc, :], w13v[:, kc, :], start=(kc == 0), stop=(kc == KC - 1)), reads=[xbT[u], wcb[w]], writes=[p1])
                        S.op("pe", lambda e: e.matmul(p3[:], xbT[u][:, kc, :], w13v[:, 8 + kc, :], start=(kc == 0), stop=(kc == KC - 1)), reads=[xbT[u], wcb[w]], writes=[p3])
                    S.op("act", lambda e: e.activation(sil[u][:], p1[:], AF.Silu), reads=[p1], writes=[sil[u]])
                    S.op("dve", lambda e: e.tensor_tensor(hid[u][:], p3[:], sil[u][:], ALU.mult), reads=[p3, sil[u]], writes=[hid[u]])

                def d2_s2a(b):
                    u = b % 2
                    pt2 = PS[2]
                    pt2b = pt2[:].bitcast(BF16)
                    for kc in range(4):
                        S.op("pe", lambda e: e.transpose(pt2b[:, kc * 128:(kc + 1) * 128], hid[u][:, kc * 128:(kc + 1) * 128], identb[:]), reads=[hid[u], identb], writes=[pt2])
                    S.op("act", lambda e: e.activation(hidT[u][:].rearrange("p k t -> p (k t)"), pt2b[:, 0:512], AF.Copy), reads=[pt2], writes=[hidT[u]])

                def d2_s2b(b):
                    u = b % 2
                    w = b % 3
                    w2v = wcb[w][:, 8192:12288].rearrange("p (a c) -> p a c", c=1024)
                    for half in range(2):
                        py = PS[5 + half]
                        for kc in range(4):
                            S.op("pe", lambda e: e.matmul(py[:], hidT[u][:, kc, :], w2v[:, kc, half * 512:(half + 1) * 512], start=(kc == 0), stop=(kc == 3)), reads=[hidT[u], wcb[w]], writes=[py])
                        if half == 0:
                            S.op("act", lambda e: e.activation(ysb[u][:, 0:512], py[:], AF.Copy), reads=[py], writes=[ysb[u]])
                        else:
                            S.op("dve", lambda e: e.tensor_copy(ysb[u][:, 512:1024], py[:]), reads=[py], writes=[ysb[u]])
                    S.dma("sp", Ys[b * 128:(b + 1) * 128, :], ysb[u][:], reads=[ysb[u]])

                d2_s1a(0)
                d2_s1a(1)
                d2_s1b(0)
                for b in range(NBLK):
                    if b + 2 < NBLK:
                        d2_s1a(b + 2)
                    d2_s2a(b)
                    if b + 1 < NBLK:
                        d2_s1b(b + 1)
                    d2_s2b(b)
                S.barrier()
            if stop_after == "D2":
                lst.close()
                break
            last = (l == nlayers - 1)
            with contextlib.ExitStack() as st:
                NBE = 4
                y1 = [sbt(st, f"y1_{i}", [128, D], F32) for i in range(NBE)]
                y2 = [sbt(st, f"y2_{i}", [128, D], F32) for i in range(NBE)]
                cmb = [sbt(st, f"cmb{i}", [128, D], F32) for i in range(NBE)]
                xt = [sbt(st, f"xt{i}", [128, KC, 128], F32) for i in range(NBE)]
                sqe = sbt(st, "sqe", [128, KC, 128], BF16)
                rse = sbt(st, "rse", [128, 128], F32)
                tme = sbt(st, "tme", [128, 128], F32)
                xo = [sbt(st, f"xo{i}", [128, KC, 128], F32) for i in range(NBE)]
                def e_a(jt):
                        u = jt % NBE
                        s = jt // (NT // NS)
                        v = vec[l][s]
                        tk = slice(jt * 128, (jt + 1) * 128)
                        S.dma("sp", xt[u][:], xw[:, :, tk].rearrange("k p t -> p k t"), writes=[xt[u]])
                        for yy, dd in ((y1[u], d1v), (y2[u], d2v)):
                            S.dma("pool", None, None, reads=[dd], writes=[yy], fn=lambda e: e.indirect_dma_start(
                                out=yy[:], out_offset=None, in_=Ys, in_offset=bass.IndirectOffsetOnAxis(ap=dd[:, jt:jt + 1], axis=0),
                                bounds_check=r_slot, oob_is_err=False))
                        S.op("dve", lambda e: e.tensor_scalar(cmb[u][:], y1[u][:], Wts[:, jt, 0:1], None, ALU.mult), reads=[y1[u], Wts], writes=[cmb[u]])
                        S.op("dve", lambda e: e.scalar_tensor_tensor(cmb[u][:], y2[u][:], Wts[:, jt, 1:2], cmb[u][:], ALU.mult, ALU.add), reads=[y2[u], Wts, cmb[u]], writes=[cmb[u]])

                def e_b(jt):
                        u = jt % NBE
                        s = jt // (NT // NS)
                        v = vec[l][s]
                        tk = slice(jt * 128, (jt + 1) * 128)
                        for half in range(2):
                            pt = nps()
                            for k4 in range(4):
                                kc = half * 4 + k4
                                S.op("pe", lambda e: e.transpose(pt[:, k4 * 128:(k4 + 1) * 128], cmb[u][:, kc * 128:(kc + 1) * 128], identf_ap), reads=[cmb[u], cst], writes=[pt])
                            for k4 in range(4):
                                kc = half * 4 + k4
                                S.op("dve", lambda e: e.scalar_tensor_tensor(xt[u][:, kc, :], pt[:, k4 * 128:(k4 + 1) * 128], v[:, 5, kc:kc + 1], xt[u][:, kc, :], ALU.mult, ALU.add), reads=[pt, xt[u]], writes=[xt[u]])
                        if not last:
                            S.dma("act", xw[:, :, tk].rearrange("k p t -> p k t"), xt[u][:], reads=[xt[u]])
                        else:
                            S.op("act", lambda e: e.activation(sqe[:], xt[u][:], AF.Square), reads=[xt[u]], writes=[sqe])
                            ssp = nps()
                            for kc in range(KC):
                                S.op("pe", lambda e: e.matmul(ssp[:, 0:128], onesb[:], sqe[:, kc, :], start=(kc == 0), stop=(kc == KC - 1)), reads=[onesb, sqe], writes=[ssp])
                            S.op("act", lambda e: e.activation(rse[:], ssp[:, 0:128], AF.Sqrt, bias=epsc[:], scale=1.0 / D), reads=[ssp, epsc], writes=[rse])
                            S.op("dve", lambda e: e.reciprocal(rse[:], rse[:]), reads=[rse], writes=[rse])
                            for kc in range(KC):
                                S.op("dve", lambda e: e.scalar_tensor_tensor(xo[u][:, kc, :], xt[u][:, kc, :], fgt[:, kc:kc + 1], rse[:], ALU.mult, ALU.mult), reads=[xt[u], fgt, rse], writes=[xo[u]])
                            S.dma("act", outT[:, :, tk].rearrange("k p t -> p k t"), xo[u][:], reads=[xo[u]])

                e_a(0)
                for jt in range(NT):
                    if jt + 1 < NT:
                        e_a(jt + 1)
                    e_b(jt)
                S.barrier()
            lst.close()
        S.barrier()
        print("bass program: ninst", S.ninst, S.cnt, flush=True)
    return nc


def _kc(a):
    k = a.shape[0] // 128
    return np.ascontiguousarray(a.reshape(k, 128, *a.shape[1:]).swapaxes(0, 1))


def make_consts():
    c = np.zeros((128, C_N), np.float32)
    c[:, C_ID:C_ID + 128] = np.eye(128, dtype=np.float32)
    k = np.arange(128)[:, None]
    q = np.arange(128)[None, :]
    c[:, C_TRI:C_TRI + 128] = np.where(k <= q, 0.0, -1e9)
    c[:, C_US:C_US + 128] = (k < q).astype(np.float32)
    for h in range(4):
        lg = np.log1p(-2.0 ** (-5.0 - h))
        rel = (q - k).astype(np.float64)
        dm = np.where(rel >= 0, np.exp(lg * np.maximum(rel, 0.0)), 0.0) * 0.125
        c[:, C_DM + h * 128:C_DM + (h + 1) * 128] = dm
        c[:, C_DI + h * 128:C_DI + (h + 1) * 128] = np.exp(lg * (np.arange(128) + 1.0))[None, :]
        c[:, C_DO + h] = np.exp(lg * (127.0 - np.arange(128))) * 0.125
    inv = (10000.0 ** (-np.arange(0, 64, 2, dtype=np.float32) / 64)).astype(np.float32)
    r = np.arange(128)
    c[:, C_INVF] = inv[r % 32]
    c[:, C_SGN] = np.where((r % 64) < 32, -1.0, 1.0)
    c[:, C_IOTA] = r
    c[:, C_THR:C_THR + NBLK] = (128.0 * np.arange(NBLK))[None, :]
    return c


def _swap64(a):
    n = a.shape[1] // 64
    b = a.reshape(a.shape[0], n, 2, 32)[:, :, ::-1, :]
    return b.reshape(a.shape[0], n * 64)


def prep_shared(inp):
    sh = {"consts": make_consts(), "fg": np.ascontiguousarray(inp["final_g"].reshape(KC, 128).T)}
    f32 = np.float32
    for l in range(L):
        w_in = inp["w_in"][l]
        o = np.cumsum([0, 256, 256, 512, 512, 512, 512, 512, 4, 256, 128, 64])
        rq, rk, rv, rg, fq, fk, fv, ff, mq, mkv, mkr = [w_in[:, o[i]:o[i + 1]] for i in range(11)]
        WA = np.concatenate([rq, _swap64(rq), rk, _swap64(rk), fq, fk, mq, mkv, mkr, _swap64(mkr), ff], axis=1)
        WB = np.concatenate([rv, rg, fv], axis=1)
        assert WA.shape[1] == WA_COLS and WB.shape[1] == WB_COLS
        wq = inp["mla_wq_up"][l].reshape(256, 4, 192)
        qn = wq[:, :, :128].reshape(256, 512)
        qp = wq[:, :, 128:].reshape(256, 256)
        wqc = np.concatenate([qn, qp, _swap64(qp)], axis=1)
        wkv = inp["mla_wkv_up"][l].reshape(128, 4, 256)
        wkvc = np.concatenate([wkv[:, :, :128].reshape(128, 512), wkv[:, :, 128:].reshape(128, 512)], axis=1)
        w1 = inp["exp_w1"][l].reshape(NE, KC, 128, 512).transpose(0, 2, 1, 3)
        w3 = inp["exp_w3"][l].reshape(NE, KC, 128, 512).transpose(0, 2, 1, 3)
        W13 = np.stack([w1, w3], axis=2)
        W2 = inp["exp_w2"][l].reshape(NE, 4, 128, 1024).transpose(0, 2, 1, 3)
        sh.update({
            f"adaw{l}": _kc(inp["ada_w"][l]), f"adab{l}": np.ascontiguousarray(inp["ada_b"][l].reshape(48, 128).T),
            f"n1g{l}": np.ascontiguousarray(inp["norm1_g"][l].reshape(KC, 128).T), f"n2g{l}": np.ascontiguousarray(inp["norm2_g"][l].reshape(KC, 128).T),
            f"WA{l}": _kc(WA), f"WB{l}": _kc(WB), f"fb{l}": np.ascontiguousarray(inp["fox_fb"][l].reshape(4, 1)),
            f"gq{l}": np.ascontiguousarray(inp["mla_q_norm_g"][l].reshape(2, 128).T), f"wq{l}": _kc(wqc),
            f"gkv{l}": np.ascontiguousarray(inp["mla_kv_norm_g"][l].reshape(128, 1)), f"wkv{l}": np.ascontiguousarray(wkvc),
            f"Wg{l}": _kc(inp["gate_w"][l]), f"gb{l}": np.ascontiguousarray(inp["gate_b"][l].reshape(24, 128).T),
            f"Wbr{l}": np.ascontiguousarray(inp["branch_w"][l].reshape(12, 128, D).swapaxes(0, 1)),
            f"Wo{l}": _kc(inp["out_w"][l]),
            f"Wr{l}": _kc(np.concatenate([inp["router_grp_w"][l], inp["router_exp_w"][l]], axis=1)),
            f"br{l}": np.ascontiguousarray(np.broadcast_to(np.concatenate([inp["router_grp_b"][l], inp["router_exp_b"][l]])[None, :], (128, 36))),
            f"W13{l}": np.ascontiguousarray(W13).reshape(NE * 128 * 4, 2048),
            f"W2{l}": np.ascontiguousarray(W2).reshape(NE * 128 * 2, 2048),
        })
    return {k: np.ascontiguousarray(v, dtype=f32) for k, v in sh.items()}


def prep_core(inp, core):
    b0 = core * NS
    x = inp["x"][b0:b0 + NS]
    xT = np.ascontiguousarray(x.transpose(2, 0, 1).reshape(KC, 128, T))
    c = inp["c"][b0:b0 + NS]
    cT = np.ascontiguousarray(c.reshape(NS, KC, 128).transpose(2, 1, 0))
    pos = np.ascontiguousarray(inp["positions"][b0:b0 + NS].reshape(1, T).astype(np.int32))
    return {"xT": xT.astype(np.float32), "cT": cT.astype(np.float32), "pos": pos}


def kernel(**inputs):
    inp = {k: np.asarray(v) for k, v in inputs.items()}
    shared = prep_shared(inp)
    nc = build()
    in_maps = []
    for c in range(NCORE):
        m = dict(shared)
        m.update(prep_core(inp, c))
        in_maps.append(m)
    res = run_bass_kernel_spmd(nc, in_maps, core_ids=list(range(NCORE)))
    out = np.empty((NCORE * NS, SEQ, D), np.float32)
    for c in range(NCORE):
        o = np.asarray(res.results[c]["outT"]).reshape(D, NS, SEQ)
        out[c * NS:(c + 1) * NS] = o.transpose(1, 2, 0)
    return out
```

```python
import contextlib
import math
import os
import numpy as np
import concourse.bass as bass
import concourse.mybir as mybir
from concourse.bass_utils import run_bass_kernel_spmd

F32 = mybir.dt.float32
BF16 = mybir.dt.bfloat16
I32 = mybir.dt.int32
AF = mybir.ActivationFunctionType
ALU = mybir.AluOpType
AX = mybir.AxisListType

NCORE = 8
L = 2
D = 1024
KC = 8
SEQ = 4096
NS = 2
T = NS * SEQ
G = 512
NG = T // G
NT = T // 128
NE = 32
NBLK = T * 2 // 128 + NE
NSLOT = NBLK * 128
EPS = 1e-6
WA_COLS = 2564
WB_COLS = 1536
SC_FOX = 128 ** -0.5
SC_MLA = 192 ** -0.5
SKIP_OFF = float(1 << 20)

C_ID = 0
C_TRI = 128
C_US = 256
C_DM = 384
C_DI = 896
C_DO = 1408
C_INVF = 1412
C_SGN = 1413
C_IOTA = 1414
C_THR = 1415
C_N = 1575

EPOCH = 30000
NDMA_SLOTS = 16


class Buf:
    __slots__ = ("ap", "lw", "rd", "excl")

    def __init__(self, ap=None, excl=False):
        self.ap = ap
        self.lw = None
        self.rd = {}
        self.excl = excl

    def __getitem__(self, idx):
        return self.ap[idx]


class Sched:
    def __init__(self, nc, stack, same_engine_sync=True):
        self.nc = nc
        self.stack = stack
        self.same = same_engine_sync
        self.engs = {"pe": nc.tensor, "act": nc.scalar, "dve": nc.vector, "pool": nc.gpsimd, "sp": nc.sync}
        self.cnt = {k: 0 for k in self.engs}
        self.esems = {k: [] for k in self.engs}
        self.waited = {k: {} for k in self.engs}
        self.dq = {}
        for q in ("sp", "act", "pool"):
            ns = NDMA_SLOTS
            sems = [self._newsem(f"dq_{q}_{i}") for i in range(ns)]
            self.dq[q] = {"sems": sems, "n": 0, "ns": ns}
        self.ninst = 0

    def _newsem(self, name):
        return self.stack.enter_context(self.nc.semaphore(name))

    def _esem(self, eng, epoch):
        lst = self.esems[eng]
        while len(lst) <= epoch:
            lst.append(self._newsem(f"e_{eng}_{len(lst)}"))
        return lst[epoch]

    def _wait(self, eng, sem, val):
        w = self.waited[eng]
        if w.get(id(sem), 0) >= val:
            return
        self.engs[eng].wait_ge(sem, val)
        w[id(sem)] = val

    def _deps(self, eng, reads, writes):
        toks = []
        for b in reads:
            if b.lw is not None:
                toks.append(b.lw)
        for b in writes:
            if b.lw is not None:
                toks.append(b.lw)
            toks.extend(b.rd.values())
        for t in toks:
            if t[2] == eng and (eng == "pe" or not self.same):
                continue
            self._wait(eng, t[0], t[1])

    def _mark(self, tok, reads, writes):
        for b in writes:
            b.lw = tok
            b.rd = {}
        for b in reads:
            if b not in writes:
                b.rd[id(tok[0])] = tok

    def op(self, eng, fn, reads=(), writes=()):
        ex = [b for b in reads if b.excl]
        if ex:
            writes = list(writes) + [b for b in ex if b not in writes]
            reads = [b for b in reads if not b.excl]
        self._deps(eng, reads, writes)
        n = self.cnt[eng]
        epoch, val = divmod(n, EPOCH)
        sem = self._esem(eng, epoch)
        inst = fn(self.engs[eng])
        inst.then_inc(sem, 1)
        self.cnt[eng] = n + 1
        self._mark((sem, val + 1, eng), reads, writes)
        self.ninst += 1
        return inst

    def dma(self, q, out, in_, reads=(), writes=(), fn=None, **kw):
        self._deps(q, reads, writes)
        st = self.dq[q]
        i = st["n"]
        ns = st["ns"]
        slot, rnd = i % ns, i // ns
        sem = st["sems"][slot]
        if rnd > 0:
            self._wait(q, sem, 16 * rnd)
        if fn is not None:
            inst = fn(self.engs[q])
        else:
            inst = self.engs[q].dma_start(out=out, in_=in_, **kw)
        inst.then_inc(sem, 16)
        st["n"] = i + 1
        self._mark((sem, 16 * (rnd + 1), "dma_" + q), reads, writes)
        self.ninst += 1
        return inst

    def barrier(self):
        toks = []
        for e, c in self.cnt.items():
            if c > 0:
                epoch, val = divmod(c - 1, EPOCH)
                toks.append((self.esems[e][epoch], val + 1))
        for q, st in self.dq.items():
            n = st["n"]
            ns = st["ns"]
            for slot in range(ns):
                issued = (n + ns - 1 - slot) // ns
                if issued > 0:
                    toks.append((st["sems"][slot], 16 * issued))
        for e in self.engs:
            for t in toks:
                self._wait(e, t[0], t[1])


def build(nlayers=L, dbg=(), stop_after=None):
    nc = bass.Bass("TRN2", target_bir_lowering=False)
    dbg = set(dbg)

    def din(name, shape, dt):
        return nc.dram_tensor(name, list(shape), dt, kind="ExternalInput").ap()

    def dscr(name, shape, dt):
        kind = "ExternalOutput" if name in dbg else "Internal"
        return nc.dram_tensor(name, list(shape), dt, kind=kind).ap()

    xT_in = din("xT", [KC, 128, T], F32)
    cT_in = din("cT", [128, KC, NS], F32)
    pos_in = din("pos", [1, T], I32)
    consts_in = din("consts", [128, C_N], F32)
    fg_in = din("fg", [128, KC], F32)
    W = []
    for l in range(L):
        W.append(dict(
            adaw=din(f"adaw{l}", [128, KC, 6 * D], F32), adab=din(f"adab{l}", [128, 48], F32),
            n1g=din(f"n1g{l}", [128, KC], F32), n2g=din(f"n2g{l}", [128, KC], F32),
            WA=din(f"WA{l}", [128, KC, WA_COLS], F32), WB=din(f"WB{l}", [128, KC, WB_COLS], F32),
            fb=din(f"fb{l}", [4, 1], F32), gq=din(f"gq{l}", [128, 2], F32), wq=din(f"wq{l}", [128, 2, 1024], F32),
            gkv=din(f"gkv{l}", [128, 1], F32), wkv=din(f"wkv{l}", [128, 1024], F32),
            Wg=din(f"Wg{l}", [128, KC, 3 * D], F32), gb=din(f"gb{l}", [128, 24], F32),
            Wbr=din(f"Wbr{l}", [128, 12, D], F32), Wo=din(f"Wo{l}", [128, KC, D], F32),
            Wr=din(f"Wr{l}", [128, KC, 36], F32), br=din(f"br{l}", [128, 36], F32),
            W13=din(f"W13{l}", [NE * 128 * 4, 2048], F32), W2=din(f"W2{l}", [NE * 128 * 2, 2048], F32),
        ))
    outT = nc.dram_tensor("outT", [KC, 128, T], F32, kind="ExternalOutput").ap()

    xw = dscr("xw", [KC, 128, T], F32)
    hTd = dscr("hTd", [KC, 128, T], BF16)
    fqT = dscr("fqT", [4, 128, T], BF16)
    fkT = dscr("fkT", [4, 128, T], BF16)
    fvd = dscr("fvd", [T, 512], BF16)
    F32d = dscr("F32d", [4, T], F32)
    F3d = dscr("F3d", [4, 3, T], BF16)
    mqnT = dscr("mqnT", [4, 128, T], BF16)
    mknT = dscr("mknT", [4, 128, T], BF16)
    mqpT = dscr("mqpT", [4, 64, T], BF16)
    kpeT = dscr("kpeT", [64, T], BF16)
    mvd = dscr("mvd", [T, 512], BF16)
    yTd = dscr("yTd", [3, 4, 128, T], BF16)
    h2tok = dscr("h2tok", [T, D], BF16)
    xs = dscr("xs", [NSLOT, D], BF16)
    Ys = dscr("Ys", [NSLOT, D], F32)
    rdbg = dscr("rdbg", [128, NT * 4], F32)
    Wcb = dscr("Wcb", [NE * 128, 12288], BF16)

    with contextlib.ExitStack() as top:
        S = Sched(nc, top)

        uid = [0]

        def sbt(stack, name, shape, dt):
            uid[0] += 1
            return Buf(stack.enter_context(nc.sbuf_tensor(f"sb{uid[0]}_{name}", list(shape), dt)))

        PS = [Buf(top.enter_context(nc.psum_tensor(f"ps{i}", [128, 512], F32)), excl=True) for i in range(8)]
        psrr = {}
        psrange = [0, 8]

        def nps(lo=None, hi=None):
            lo = psrange[0] if lo is None else lo
            hi = psrange[1] if hi is None else hi
            i = psrr.get((lo, hi), 0)
            psrr[(lo, hi)] = (i + 1) % (hi - lo)
            return PS[lo + i]

        r_slot = top.enter_context(nc.gpsimd.register("r_slot"))
        r_w13 = top.enter_context(nc.gpsimd.register("r_w13"))
        r_w2 = top.enter_context(nc.gpsimd.register("r_w2"))
        nc.gpsimd.reg_mov(r_slot, NSLOT - 1)
        nc.gpsimd.reg_mov(r_w13, NE * 128 * 4 - 1)
        nc.gpsimd.reg_mov(r_w2, NE * 128 * 2 - 1)
        r_e = top.enter_context(nc.gpsimd.register("r_e"))
        nc.gpsimd.reg_mov(r_e, NE * 128 - 1)
        cst = sbt(top, "cst", [128, C_N], F32)
        S.dma("sp", cst[:], consts_in, writes=[cst])
        identb = sbt(top, "identb", [128, 128], BF16)
        onesb = sbt(top, "onesb", [128, 128], BF16)
        negtri = sbt(top, "negtri", [128, 128], BF16)
        ustr = sbt(top, "ustr", [128, 128], BF16)
        epsc = sbt(top, "epsc", [128, 1], F32)
        onec = sbt(top, "onec", [128, 1], F32)
        ones512 = sbt(top, "ones512", [4, 512], F32)
        S.op("dve", lambda e: e.tensor_copy(identb[:], cst[:, C_ID:C_ID + 128]), reads=[cst], writes=[identb])
        S.op("dve", lambda e: e.memset(onesb[:], 1.0), writes=[onesb])
        S.op("dve", lambda e: e.tensor_copy(negtri[:], cst[:, C_TRI:C_TRI + 128]), reads=[cst], writes=[negtri])
        S.op("dve", lambda e: e.tensor_copy(ustr[:], cst[:, C_US:C_US + 128]), reads=[cst], writes=[ustr])
        S.op("dve", lambda e: e.memset(epsc[:], EPS), writes=[epsc])
        S.op("dve", lambda e: e.memset(onec[:], 1.0), writes=[onec])
        S.op("dve", lambda e: e.memset(ones512[:], 1.0), writes=[ones512])
        identf_ap = cst[:, C_ID:C_ID + 128]

        vec = [[sbt(top, f"vec{l}_{s}", [128, 6, KC], F32) for s in range(NS)] for l in range(L)]
        fgt = sbt(top, "fgt", [128, KC], F32)
        S.dma("sp", fgt[:], fg_in, writes=[fgt])

        with contextlib.ExitStack() as st:
            cT = sbt(st, "cT", [128, KC, NS], F32)
            cact = sbt(st, "cact", [128, KC, NS], F32)
            S.dma("sp", cT[:], cT_in, writes=[cT])
            S.op("act", lambda e: e.activation(cact[:], cT[:], AF.Silu), reads=[cT], writes=[cact])
            wblk = [sbt(st, f"wblk{i}", [128, KC, 512], F32) for i in range(2)]
            modt = sbt(st, "modt", [128, 48, NS], F32)
            adab = sbt(st, "adab", [128, 48], F32)
            ng = sbt(st, "ng", [128, 2, KC], F32)
            for l in range(nlayers):
                S.dma("sp", adab[:], W[l]["adab"], writes=[adab])
                S.dma("sp", ng[:, 0, :], W[l]["n1g"], writes=[ng])
                S.dma("sp", ng[:, 1, :], W[l]["n2g"], writes=[ng])
                mps = nps()
                for cb in range(12):
                    wb = wblk[cb % 2]
                    S.dma("sp" if cb % 2 == 0 else "act", wb[:], W[l]["adaw"][:, :, cb * 512:(cb + 1) * 512], writes=[wb])
                    for oc in range(4):
                        j = cb * 4 + oc
                        for kc in range(KC):
                            S.op("pe", lambda e: e.matmul(mps[:, j * 2:(j + 1) * 2], wb[:, kc, oc * 128:(oc + 1) * 128], cact[:, kc, :],
                                                          start=(kc == 0), stop=(kc == KC - 1)), reads=[wb, cact], writes=[mps])
                for s in range(NS):
                    S.op("dve", lambda e: e.tensor_tensor(modt[:, :, s], mps[:, 0:96].rearrange("p (j s) -> p j s", s=2)[:, :, s], adab[:], ALU.add),
                         reads=[mps, adab], writes=[modt])
                for s in range(NS):
                    v = vec[l][s]
                    S.op("dve", lambda e: e.scalar_tensor_tensor(v[:, 0, :], modt[:, 8:16, s], 1.0, ng[:, 0, :], ALU.add, ALU.mult), reads=[modt, ng], writes=[v])
                    S.op("dve", lambda e: e.tensor_copy(v[:, 1, :], modt[:, 0:8, s]), reads=[modt], writes=[v])
                    S.op("dve", lambda e: e.tensor_copy(v[:, 2, :], modt[:, 16:24, s]), reads=[modt], writes=[v])
                    S.op("dve", lambda e: e.scalar_tensor_tensor(v[:, 3, :], modt[:, 32:40, s], 1.0, ng[:, 1, :], ALU.add, ALU.mult), reads=[modt, ng], writes=[v])
                    S.op("dve", lambda e: e.tensor_copy(v[:, 4, :], modt[:, 24:32, s]), reads=[modt], writes=[v])
                    S.op("dve", lambda e: e.tensor_copy(v[:, 5, :], modt[:, 40:48, s]), reads=[modt], writes=[v])
            S.barrier()
        cut = int(os.environ.get("KCUT", "99"))
        if cut == 0:
            return nc

        def norm_mod(x32, n, sq, rstd, tmp, acol, bcol, out_bf=None, out_f32=None):
            S.op("act", lambda e: e.activation(sq[:, :, 0:n], x32[:, :, 0:n], AF.Square), reads=[x32], writes=[sq])
            ssp = nps()
            for kc in range(KC):
                S.op("pe", lambda e: e.matmul(ssp[:, 0:n], onesb[:], sq[:, kc, 0:n], start=(kc == 0), stop=(kc == KC - 1)),
                     reads=[onesb, sq], writes=[ssp])
            S.op("act", lambda e: e.activation(rstd[:, 0:n], ssp[:, 0:n], AF.Sqrt, bias=epsc[:], scale=1.0 / D), reads=[ssp, epsc], writes=[rstd])
            S.op("dve", lambda e: e.reciprocal(rstd[:, 0:n], rstd[:, 0:n]), reads=[rstd], writes=[rstd])
            for kc in range(KC):
                S.op("dve", lambda e: e.scalar_tensor_tensor(tmp[:, 0:n], x32[:, kc, 0:n], acol(kc), rstd[:, 0:n], ALU.mult, ALU.mult),
                     reads=[x32, rstd], writes=[tmp])
                if out_f32 is not None:
                    S.op("act", lambda e: e.activation(out_f32[:, kc, 0:n], tmp[:, 0:n], AF.Identity, bias=bcol(kc), scale=1.0), reads=[tmp], writes=[out_f32])
                    if out_bf is not None:
                        S.op("pool", lambda e: e.tensor_copy(out_bf[:, kc, 0:n], out_f32[:, kc, 0:n]), reads=[out_f32], writes=[out_bf])
                else:
                    S.op("act", lambda e: e.activation(out_bf[:, kc, 0:n], tmp[:, 0:n], AF.Identity, bias=bcol(kc), scale=1.0), reads=[tmp], writes=[out_bf])

        def wload(stack, name, shape, src, q="pool", defer=False, after=()):
            t = sbt(stack, name, shape, BF16)
            if defer:
                return t
            return wfill(t, shape, src, after)

        def wfill(t, shape, src, after=()):
            after = list(after)
            if len(shape) == 3:
                for kc in range(shape[1]):
                    c = 0
                    while c < shape[2]:
                        w = min(2048, shape[2] - c)
                        S.dma("pool", t[:, kc, c:c + w], src[:, kc, c:c + w], reads=after, writes=[t])
                        c += w
            else:
                c = 0
                while c < shape[1]:
                    w = min(2048, shape[1] - c)
                    S.dma("pool", t[:, c:c + w], src[:, c:c + w], reads=after, writes=[t])
                    c += w
            return t

        for l in range(nlayers):
            Wl = W[l]
            xsrc = xT_in if l == 0 else xw
            lst = contextlib.ExitStack()
            lst.__enter__()
            Wts = sbt(lst, f"Wts_{l}", [128, NT, 2], F32)
            d1v = sbt(lst, f"d1v_{l}", [128, NT], I32)
            d2v = sbt(lst, f"d2v_{l}", [128, NT], I32)
            ibv = sbt(lst, f"ibv_{l}", [128, NBLK], I32)
            with contextlib.ExitStack() as st:
                psrange[:] = [0, 6]
                WA = wload(st, "WA", [128, KC, WA_COLS], Wl["WA"])
                WB = wload(st, "WB", [128, KC, WB_COLS], Wl["WB"])
                wq = wload(st, "wq", [128, 2, 1024], Wl["wq"])
                wkv = wload(st, "wkv", [128, 1024], Wl["wkv"])
                gq = sbt(st, "gq", [128, 2], F32)
                gkv = sbt(st, "gkv", [128, 1], F32)
                nfb = sbt(st, "nfb", [4, 1], F32)
                S.dma("sp", gq[:], Wl["gq"], writes=[gq])
                S.dma("sp", gkv[:], Wl["gkv"], writes=[gkv])
                S.dma("sp", nfb[:], Wl["fb"], writes=[nfb])
                S.op("dve", lambda e: e.tensor_scalar(nfb[:], nfb[:], -1.0, None, ALU.mult), reads=[nfb], writes=[nfb])
                x32 = sbt(st, "x32", [128, KC, G], F32)
                sq = sbt(st, "sq", [128, KC, G], BF16)
                hT = sbt(st, "hT", [128, KC, G], BF16)
                rstd = sbt(st, "rstd", [128, G], F32)
                tmp = sbt(st, "tmp", [128, G], F32)
                posi = sbt(st, "posi", [128, G], I32)
                ang = sbt(st, "ang", [128, G], F32)
                rr = sbt(st, "rr", [128, G], F32)
                ki = sbt(st, "ki", [128, G], I32)
                kf = sbt(st, "kf", [128, G], F32)
                msk = sbt(st, "msk", [128, G], F32)
                cosT = sbt(st, "cosT", [128, G], F32)
                sinT = sbt(st, "sinT", [128, G], F32)
                t1 = [sbt(st, f"t1_{i}", [128, G], F32) for i in range(2)]
                t2 = [sbt(st, f"t2_{i}", [128, G], F32) for i in range(2)]
                R2q = [sbt(st, f"R2q{p}", [128, G], BF16) for p in range(2)]
                R2k = [sbt(st, f"R2k{p}", [128, G], BF16) for p in range(2)]
                rqO = [sbt(st, f"rqO{p}", [64, G], BF16) for p in range(2)]
                rkO = [sbt(st, f"rkO{p}", [64, G], BF16) for p in range(2)]
                rqB = [R2q[0], rqO[0], R2q[1], rqO[1]]
                rkB = [R2k[0], rkO[0], R2k[1], rkO[1]]

                def rqA(h, tsl):
                    return rqB[h][0:64, tsl]

                def rkA(h, tsl):
                    return rkB[h][0:64, tsl]
                stg = [sbt(st, f"stg{i}", [128, G], BF16) for i in range(3)]
                mq32 = sbt(st, "mq32", [128, 3, G], F32)
                msq = sbt(st, "msq", [128, 3, G], BF16)
                mqn = sbt(st, "mqn", [128, 3, G], BF16)
                mrs = sbt(st, "mrs", [128, G], F32)
                rv = sbt(st, "rv", [128, 4, 512], BF16)
                rgs = sbt(st, "rgs", [128, 4, 512], F32)
                fa = sbt(st, "fa", [4, G], F32)
                Fg = sbt(st, "Fg", [4, G], F32)
                fbb = sbt(st, "fbb", [4, G], F32)
                fcar = sbt(st, "fcar", [4, 1], F32)
                f3 = [sbt(st, f"f3_{i}", [4, G], BF16) for i in range(3)]
                S32A = sbt(st, "S32A", [64, 512], F32)
                SbfA = sbt(st, "SbfA", [64, 512], BF16)
                dcT = sbt(st, "dcT", [64, 512], F32)
                for h in range(4):
                    dc = float(np.exp(128.0 * np.log1p(-2.0 ** (-5.0 - h))))
                    S.op("dve", lambda e: e.memset(dcT[:, h * 128:(h + 1) * 128], dc), writes=[dcT])
                PTa = [sbt(st, f"PTa{i}", [128, 512], BF16) for i in range(2)]
                qda = [sbt(st, f"qda{i}", [64, 4, 128], BF16) for i in range(2)]
                Kda = [sbt(st, f"Kda{i}", [128, 4, 64], BF16) for i in range(2)]
                osq = sbt(st, "osq", [128, 512], F32)
                onrm = sbt(st, "onrm", [128, 512], F32)
                yab = sbt(st, "yab", [128, 512], BF16)
                gst = sbt(st, "gst", [128, 6, 4], F32)
                yaTg = sbt(st, "yaTg", [128, 4, G], BF16)
                rot = [0]

                def rope_evac(ps_a, ps_b, dst, rows=64):
                    i = rot[0] % 2
                    rot[0] += 1
                    S.op("dve", lambda e: e.tensor_tensor(t1[i][0:rows, :], ps_a[0:rows, :], cosT[0:rows, :], ALU.mult), reads=[ps_a, cosT], writes=[t1[i]])
                    S.op("dve", lambda e: e.tensor_tensor(t2[i][0:rows, :], ps_b[0:rows, :], sinT[0:rows, :], ALU.mult), reads=[ps_b, sinT], writes=[t2[i]])
                    S.op("pool", lambda e: e.tensor_tensor(dst[0:rows, :], t1[i][0:rows, :], t2[i][0:rows, :], ALU.add), reads=[t1[i], t2[i]], writes=[dst])

                def projA(c0, m, rhsbuf=None, nk=KC, wt=None, rsel=None):
                    ps = nps()
                    wt_ = WA if wt is None else wt
                    rb = hT if rhsbuf is None else rhsbuf
                    for kc in range(nk):
                        S.op("pe", lambda e: e.matmul(ps[0:m, :], wt_[:, kc, c0:c0 + m], rb[:, kc, :], start=(kc == 0), stop=(kc == nk - 1)),
                             reads=[wt_, rb], writes=[ps])
                    return ps

                sg = [0]

                def stage_out(ps, m, dst_ap, scale=None):
                    b = stg[sg[0] % 3]
                    sg[0] += 1
                    S.op("act", lambda e: e.activation(b[0:m, :], ps[0:m, :], AF.Copy), reads=[ps], writes=[b])
                    S.dma("sp", dst_ap, b[0:m, :], reads=[b])

                for g in range(NG):
                    s = g // (NG // NS)
                    g0 = g * G
                    first = (g % (NG // NS) == 0)
                    v = vec[l][s]
                    for half in range(2):
                        S.dma("sp" if half == 0 else "act", x32[:, half * 4:(half + 1) * 4, :],
                              xsrc[half * 4:(half + 1) * 4, :, g0:g0 + G].rearrange("k p t -> p k t"), writes=[x32])
                    for ci in range(g * 3, (g + 1) * 3):
                        if ci < 32:
                            rs = slice(ci * 128, (ci + 1) * 128)
                            S.dma("pool", Wcb[rs, 0:8192].rearrange("r (j c) -> r j c", c=2048),
                                  Wl["W13"][ci * 512:(ci + 1) * 512, :].rearrange("(r j) c -> r j c", j=4))
                        else:
                            c2 = ci - 32
                            rs = slice(c2 * 256, (c2 + 1) * 256)
                            S.dma("pool", Wcb[rs, 8192:12288].rearrange("r (j c) -> r j c", c=2048),
                                  Wl["W2"][c2 * 512:(c2 + 1) * 512, :].rearrange("(r j) c -> r j c", j=2))
                    norm_mod(x32, G, sq, rstd, tmp, lambda kc: v[:, 0, kc:kc + 1], lambda kc: v[:, 1, kc:kc + 1], out_bf=hT)
                    S.dma("act", hTd[:, :, g0:g0 + G].rearrange("k p t -> p k t"), hT[:], reads=[hT])
                    if cut == 2:
                        break
                    S.dma("sp", posi[:], pos_in[0:1, g0:g0 + G].partition_broadcast(128), writes=[posi])
                    S.op("dve", lambda e: e.tensor_copy(ang[:], posi[:]), reads=[posi], writes=[ang])
                    S.op("dve", lambda e: e.tensor_scalar(ang[:], ang[:], cst[:, C_INVF:C_INVF + 1], None, ALU.mult), reads=[ang, cst], writes=[ang])
                    for which, dstT in ((0, sinT), (1, cosT)):
                        shift = 0.0 if which == 0 else math.pi / 2
                        S.op("dve", lambda e: e.tensor_scalar(rr[:], ang[:], shift, 1.0 / (2 * math.pi), ALU.add, ALU.mult), reads=[ang], writes=[rr])
                        S.op("dve", lambda e: e.tensor_copy(ki[:], rr[:]), reads=[rr], writes=[ki])
                        S.op("dve", lambda e: e.tensor_copy(kf[:], ki[:]), reads=[ki], writes=[kf])
                        S.op("dve", lambda e: e.tensor_scalar(rr[:], ang[:], shift, None, ALU.add), reads=[ang], writes=[rr])
                        S.op("dve", lambda e: e.scalar_tensor_tensor(rr[:], kf[:], -6.28125, rr[:], ALU.mult, ALU.add), reads=[kf, rr], writes=[rr])
                        S.op("dve", lambda e: e.scalar_tensor_tensor(rr[:], kf[:], -(2 * math.pi - 6.28125), rr[:], ALU.mult, ALU.add), reads=[kf, rr], writes=[rr])
                        S.op("dve", lambda e: e.tensor_scalar(msk[:], rr[:], math.pi, -2 * math.pi, ALU.is_gt, ALU.mult), reads=[rr], writes=[msk])
                        S.op("dve", lambda e: e.tensor_tensor(rr[:], rr[:], msk[:], ALU.add), reads=[rr, msk], writes=[rr])
                        S.op("dve", lambda e: e.tensor_scalar(msk[:], rr[:], -math.pi, 2 * math.pi, ALU.is_lt, ALU.mult), reads=[rr], writes=[msk])
                        S.op("dve", lambda e: e.tensor_tensor(rr[:], rr[:], msk[:], ALU.add), reads=[rr, msk], writes=[rr])
                        S.op("dve", lambda e: e.tensor_scalar(rr[:], rr[:], math.pi, -math.pi, ALU.min, ALU.max), reads=[rr], writes=[rr])
                        if which == 0:
                            S.op("act", lambda e: e.activation(dstT[:], rr[:], AF.Sin, scale=cst[:, C_SGN:C_SGN + 1]), reads=[rr, cst], writes=[dstT])
                        else:
                            S.op("act", lambda e: e.activation(dstT[:], rr[:], AF.Sin), reads=[rr], writes=[dstT])
                    if cut == 3:
                        break
                    for p in range(2):
                        pa = projA(0 + p * 128, 128)
                        pb = projA(256 + p * 128, 128)
                        rope_evac(pa, pb, R2q[p], rows=128)
                        S.dma("sp", rqO[p][:], R2q[p][64:128, :], reads=[R2q[p]], writes=[rqO[p]])
                        pa = projA(512 + p * 128, 128)
                        pb = projA(768 + p * 128, 128)
                        rope_evac(pa, pb, R2k[p], rows=128)
                        S.dma("act", rkO[p][:], R2k[p][64:128, :], reads=[R2k[p]], writes=[rkO[p]])
                    if cut == 4:
                        break
                    for h in range(4):
                        stage_out(projA(1024 + h * 128, 128), 128, fqT[h, :, g0:g0 + G])
                        stage_out(projA(1536 + h * 128, 128), 128, fkT[h, :, g0:g0 + G])
                    if cut == 5:
                        break
                    for j in range(3):
                        ps = projA(2048 + j * 128, 128)
                        S.op("act", lambda e: e.activation(mq32[:, j, :], ps[:], AF.Copy), reads=[ps], writes=[mq32])
                    S.op("act", lambda e: e.activation(msq[:], mq32[:], AF.Square), reads=[mq32], writes=[msq])
                    ssq = nps()
                    for j in range(2):
                        S.op("pe", lambda e: e.matmul(ssq[:], onesb[:], msq[:, j, :], start=(j == 0), stop=(j == 1)), reads=[onesb, msq], writes=[ssq])
                    S.op("act", lambda e: e.activation(mrs[:], ssq[:], AF.Sqrt, bias=epsc[:], scale=1.0 / 256), reads=[ssq, epsc], writes=[mrs])
                    S.op("dve", lambda e: e.reciprocal(mrs[:], mrs[:]), reads=[mrs], writes=[mrs])
                    for j in range(2):
                        S.op("dve", lambda e: e.scalar_tensor_tensor(mqn[:, j, :], mq32[:, j, :], gq[:, j:j + 1], mrs[:], ALU.mult, ALU.mult), reads=[mq32, gq, mrs], writes=[mqn])
                    ssk = nps()
                    S.op("pe", lambda e: e.matmul(ssk[:], onesb[:], msq[:, 2, :], start=True, stop=True), reads=[onesb, msq], writes=[ssk])
                    S.op("act", lambda e: e.activation(mrs[:], ssk[:], AF.Sqrt, bias=epsc[:], scale=1.0 / 128), reads=[ssk, epsc], writes=[mrs])
                    S.op("dve", lambda e: e.reciprocal(mrs[:], mrs[:]), reads=[mrs], writes=[mrs])
                    S.op("dve", lambda e: e.scalar_tensor_tensor(mqn[:, 2, :], mq32[:, 2, :], gkv[:, 0:1], mrs[:], ALU.mult, ALU.mult), reads=[mq32, gkv, mrs], writes=[mqn])
                    pa = projA(2432, 64)
                    pb = projA(2496, 64)
                    bb = stg[sg[0] % 3]
                    sg[0] += 1
                    rope_evac(pa, pb, bb)
                    S.dma("sp", kpeT[:, g0:g0 + G], bb[0:64, :], reads=[bb])
                    for h in range(4):
                        stage_out(projA(h * 128, 128, rhsbuf=mqn, nk=2, wt=wq), 128, mqnT[h, :, g0:g0 + G])
                        pa = projA(512 + h * 64, 64, rhsbuf=mqn, nk=2, wt=wq)
                        pb = projA(768 + h * 64, 64, rhsbuf=mqn, nk=2, wt=wq)
                        bb = stg[sg[0] % 3]
                        sg[0] += 1
                        rope_evac(pa, pb, bb)
                        S.dma("sp", mqpT[h, :, g0:g0 + G], bb[0:64, :], reads=[bb])
                        ps = nps()
                        S.op("pe", lambda e: e.matmul(ps[:], wkv[:, h * 128:(h + 1) * 128], mqn[:, 2, :], start=True, stop=True), reads=[wkv, mqn], writes=[ps])
                        stage_out(ps, 128, mknT[h, :, g0:g0 + G])
                    if cut == 6:
                        break
                    ps = projA(2560, 4)
                    S.op("act", lambda e: e.activation(fa[:], ps[0:4, :], AF.Exp, bias=nfb[:], scale=-1.0), reads=[ps, nfb], writes=[fa])
                    S.op("act", lambda e: e.activation(fa[:], fa[:], AF.Ln, bias=onec[0:4, :], scale=1.0), reads=[fa, onec], writes=[fa])
                    if first:
                        S.op("dve", lambda e: e.memset(fcar[:], 0.0), writes=[fcar])
                    S.op("dve", lambda e: e.tensor_tensor_scan(Fg[:], ones512[:], fa[:], fcar[:], ALU.mult, ALU.subtract), reads=[ones512, fa, fcar], writes=[Fg])
                    S.op("dve", lambda e: e.tensor_copy(fcar[:], Fg[:, G - 1:G]), reads=[Fg], writes=[fcar])
                    S.dma("sp", F32d[:, g0:g0 + G], Fg[:], reads=[Fg])
                    S.op("dve", lambda e: e.tensor_scalar(fbb[:], Fg[:], 1.0 / SC_FOX, None, ALU.mult), reads=[Fg], writes=[fbb])
                    for i in range(3):
                        S.op("dve", lambda e: e.tensor_copy(f3[i][:], fbb[:]), reads=[fbb], writes=[f3[i]])
                        if i < 2:
                            S.op("dve", lambda e: e.tensor_tensor(fbb[:], fbb[:], f3[i][:], ALU.subtract), reads=[fbb, f3[i]], writes=[fbb])
                        S.dma("sp", F3d[:, i, g0:g0 + G], f3[i][:], reads=[f3[i]])
                    if cut == 7:
                        break
                    for tt in range(4):
                        tsl = slice(tt * 128, (tt + 1) * 128)
                        for j in range(3):
                            ps = nps()
                            for kc in range(KC):
                                S.op("pe", lambda e: e.matmul(ps[:], hT[:, kc, tsl], WB[:, kc, j * 512:(j + 1) * 512], start=(kc == 0), stop=(kc == KC - 1)),
                                     reads=[hT, WB], writes=[ps])
                            if j == 0:
                                S.op("act", lambda e: e.activation(rv[:, tt, :], ps[:], AF.Copy), reads=[ps], writes=[rv])
                            elif j == 1:
                                S.op("act", lambda e: e.activation(rgs[:, tt, :], ps[:], AF.Silu), reads=[ps], writes=[rgs])
                            else:
                                stage_out(ps, 128, fvd[g0 + tt * 128:g0 + (tt + 1) * 128, :])
                        ps = nps()
                        S.op("pe", lambda e: e.matmul(ps[:], mqn[:, 2, tsl], wkv[:, 512:1024], start=True, stop=True), reads=[mqn, wkv], writes=[ps])
                        stage_out(ps, 128, mvd[g0 + tt * 128:g0 + (tt + 1) * 128, :])
                    if cut == 8:
                        break
                    if first:
                        S.op("dve", lambda e: e.memset(S32A[:], 0.0), writes=[S32A])
                        S.op("dve", lambda e: e.memset(SbfA[:], 0.0), writes=[SbfA])
                    pend_norm = [None]
                    for tt in (range(4) if os.environ.get("KSKIPRET") is None else []):
                        tsl = slice(tt * 128, (tt + 1) * 128)
                        pso = nps(6, 8)
                        u2 = tt % 2
                        pss = nps()
                        psk = nps()
                        pskb = psk[:].bitcast(BF16)
                        for h in range(4):
                            S.op("pe", lambda e: e.matmul(pss[:, h * 128:(h + 1) * 128], rkA(h, tsl), rqA(h, tsl), start=True, stop=True), reads=[rkB[h], rqB[h]], writes=[pss])
                        for h in range(4):
                            S.op("pe", lambda e: e.transpose(pskb[:, h * 64:(h + 1) * 64], rkA(h, tsl), identb[0:64, 0:64]), reads=[rkB[h], identb], writes=[psk])
                        S.op("dve", lambda e: e.tensor_tensor(PTa[u2][:], pss[:], cst[:, C_DM:C_DM + 512], ALU.mult), reads=[pss, cst], writes=[PTa[u2]])
                        S.op("dve", lambda e: e.tensor_tensor(Kda[u2][:], pskb[:, 0:256].rearrange("p (h d) -> p h d", h=4),
                                                              cst[:, C_DO:C_DO + 4].unsqueeze(2).to_broadcast([128, 4, 64]), ALU.mult), reads=[psk, cst], writes=[Kda[u2]])
                        for h in range(4):
                            S.op("pool", lambda e: e.tensor_tensor(qda[u2][:, h, :], rqA(h, tsl), cst[0:64, C_DI + h * 128:C_DI + (h + 1) * 128], ALU.mult),
                                 reads=[rqB[h], cst], writes=[qda[u2]])
                        for h in range(4):
                            hs = slice(h * 128, (h + 1) * 128)
                            S.op("pe", lambda e: e.matmul(pso[:, hs], PTa[u2][:, hs], rv[:, tt, hs], start=True, stop=False), reads=[PTa[u2], rv], writes=[pso])
                            S.op("pe", lambda e: e.matmul(pso[:, hs], qda[u2][:, h, :], SbfA[:, hs], start=False, stop=True), reads=[qda[u2], SbfA], writes=[pso])
                        pkv = nps()
                        for h in range(4):
                            hs = slice(h * 128, (h + 1) * 128)
                            S.op("pe", lambda e: e.matmul(pkv[0:64, hs], Kda[u2][:, h, :], rv[:, tt, hs], start=True, stop=True), reads=[Kda[u2], rv], writes=[pkv])
                        S.op("dve", lambda e: e.tensor_tensor(S32A[:], S32A[:], dcT[:], ALU.mult), reads=[S32A, dcT], writes=[S32A])
                        S.op("dve", lambda e: e.tensor_tensor(S32A[:], S32A[:], pkv[0:64, :], ALU.add), reads=[S32A, pkv], writes=[S32A])
                        S.op("pool", lambda e: e.tensor_copy(SbfA[:], S32A[:]), reads=[S32A], writes=[SbfA])
                        def ret_norm(pso=pso, tt=tt, tsl=tsl):
                            pv = pso[:].rearrange("p (h e) -> p h e", h=4)
                            S.op("dve", lambda e: e.tensor_reduce(gst[:, 0, :], pv, AX.X, ALU.add), reads=[pso], writes=[gst])
                            S.op("act", lambda e: e.activation(osq[:], pso[:], AF.Square), reads=[pso], writes=[osq])
                            S.op("dve", lambda e: e.tensor_reduce(gst[:, 1, :], osq[:].rearrange("p (h e) -> p h e", h=4), AX.X, ALU.add), reads=[osq], writes=[gst])
                            S.op("dve", lambda e: e.tensor_scalar(gst[:, 2, :], gst[:, 0, :], 1.0 / 128, None, ALU.mult), reads=[gst], writes=[gst])
                            S.op("dve", lambda e: e.tensor_tensor(gst[:, 3, :], gst[:, 2, :], gst[:, 2, :], ALU.mult), reads=[gst], writes=[gst])
                            S.op("dve", lambda e: e.scalar_tensor_tensor(gst[:, 3, :], gst[:, 1, :], 1.0 / 128, gst[:, 3, :], ALU.mult, ALU.subtract), reads=[gst], writes=[gst])
                            S.op("act", lambda e: e.activation(gst[:, 4, :], gst[:, 3, :], AF.Sqrt, bias=epsc[:], scale=1.0), reads=[gst, epsc], writes=[gst])
                            S.op("dve", lambda e: e.reciprocal(gst[:, 4, :], gst[:, 4, :]), reads=[gst], writes=[gst])
                            S.op("dve", lambda e: e.scalar_tensor_tensor(gst[:, 5, :], gst[:, 2, :], -1.0, gst[:, 4, :], ALU.mult, ALU.mult), reads=[gst], writes=[gst])
                            for h in range(4):
                                hs = slice(h * 128, (h + 1) * 128)
                                S.op("act", lambda e: e.activation(onrm[:, hs], pso[:, hs], AF.Identity, bias=gst[:, 5, h:h + 1], scale=gst[:, 4, h:h + 1]), reads=[pso, gst], writes=[onrm])
                            S.op("pool", lambda e: e.tensor_tensor(yab[:], onrm[:], rgs[:, tt, :], ALU.mult), reads=[onrm, rgs], writes=[yab])
                            pst = nps()
                            pstb = pst[:].bitcast(BF16)
                            for kc in range(4):
                                S.op("pe", lambda e: e.transpose(pstb[:, kc * 128:(kc + 1) * 128], yab[:, kc * 128:(kc + 1) * 128], identb[:]), reads=[yab, identb], writes=[pst])
                            S.op("act", lambda e: e.activation(yaTg[:, :, tsl], pstb[:, 0:512].rearrange("p (k t) -> p k t", k=4), AF.Copy), reads=[pst], writes=[yaTg])

                        if pend_norm[0] is not None:
                            pend_norm[0]()
                        pend_norm[0] = ret_norm
                    if pend_norm[0] is not None:
                        pend_norm[0]()
                    pend_norm[0] = None
                    if cut in (9, 10, 11):
                        break
                    S.dma("act", yTd[0, :, :, g0:g0 + G].rearrange("k p t -> p k t"), yaTg[:], reads=[yaTg])
                S.barrier()
            if stop_after == "A":
                lst.close()
                break

            psrange[:] = [0, 8]
            cwst = contextlib.ExitStack()
            cwst.__enter__()
            Wg = wload(cwst, "Wg", [128, KC, 3 * D], Wl["Wg"], defer=True)
            Wbr = wload(cwst, "Wbr", [128, 12, D], Wl["Wbr"], defer=True)
            Wo = wload(cwst, "Wo", [128, KC, D], Wl["Wo"], defer=True)
            with contextlib.ExitStack() as st:
                NB = 2
                QT = [sbt(st, f"QT{i}", [128, SEQ], BF16) for i in range(NB)]
                KT = [sbt(st, f"KT{i}", [128, SEQ], BF16) for i in range(NB)]
                VV = [sbt(st, f"VV{i}", [128, 32, 128], BF16) for i in range(NB)]
                XQ = [sbt(st, f"XQ{i}", [64, SEQ], BF16) for i in range(NB)]
                XK = [sbt(st, f"XK{i}", [64, SEQ], BF16) for i in range(NB)]
                nFk = [sbt(st, f"nFk{i}", [128, 32], F32) for i in range(NB)]
                yTs = [sbt(st, f"yTs{i}", [128, SEQ], BF16) for i in range(NB)]
                PT = [sbt(st, f"PT{i}", [128, 512], BF16) for i in range(4)]
                rec = sbt(st, "rec", [128, 512], F32)
                zero_c = sbt(st, "zero_c", [128, 1], F32)
                S.op("dve", lambda e: e.memset(zero_c[:], 0.0), writes=[zero_c])
                SP = [PS[0], PS[1], PS[2], PS[3]]
                OP = [PS[4], PS[5]]
                RP = [PS[6], PS[7]]
                unit = 0
                for kind in ("fox", "mla"):
                    for s in range(NS):
                        for h in range(4):
                            u = unit % NB
                            unit += 1
                            s0 = s * SEQ
                            if kind == "fox":
                                S.dma("sp", QT[u][:], fqT[h, :, s0:s0 + SEQ], writes=[QT[u]])
                                S.dma("act", KT[u][:], fkT[h, :, s0:s0 + SEQ], writes=[KT[u]])
                                S.dma("sp", VV[u][:], fvd[s0:s0 + SEQ, h * 128:(h + 1) * 128].rearrange("(b p) e -> p b e", p=128), writes=[VV[u]])
                                S.dma("act", XQ[u][0:3, :], F3d[h, :, s0:s0 + SEQ], writes=[XQ[u]])
                                with nc.allow_non_contiguous_dma(reason="small forget-bias transpose load"):
                                    S.dma("sp", nFk[u][:], F32d[h, s0:s0 + SEQ].rearrange("(b p) -> p b", p=128), writes=[nFk[u]])
                                S.op("dve", lambda e: e.tensor_scalar(nFk[u][:], nFk[u][:], -1.0, None, ALU.mult), reads=[nFk[u]], writes=[nFk[u]])
                                sc = SC_FOX
                                ybr = 1
                            else:
                                S.dma("sp", QT[u][:], mqnT[h, :, s0:s0 + SEQ], writes=[QT[u]])
                                S.dma("act", KT[u][:], mknT[h, :, s0:s0 + SEQ], writes=[KT[u]])
                                S.dma("sp", VV[u][:], mvd[s0:s0 + SEQ, h * 128:(h + 1) * 128].rearrange("(b p) e -> p b e", p=128), writes=[VV[u]])
                                S.dma("act", XQ[u][:], mqpT[h, :, s0:s0 + SEQ], writes=[XQ[u]])
                                S.dma("sp", XK[u][:], kpeT[:, s0:s0 + SEQ], writes=[XK[u]])
                                sc = SC_MLA
                                ybr = 2
                            pairs = [(g, kb) for g in range(8) for kb in range(4 * g + 4)]

                            def emit_S(idx):
                                g, kb = pairs[idx]
                                j = kb - 4 * g
                                c0 = max(j, 0) * 128
                                sp_ = SP[idx % 4]
                                q0 = g * 512 + c0
                                q1 = (g + 1) * 512
                                ksl = slice(kb * 128, (kb + 1) * 128)
                                S.op("pe", lambda e: e.matmul(sp_[:, c0:512], KT[u][:, ksl], QT[u][:, q0:q1], start=True, stop=False), reads=[KT[u], QT[u]], writes=[sp_])
                                last2 = (j < 0)
                                if kind == "fox":
                                    S.op("pe", lambda e: e.matmul(sp_[:, c0:512], onesb[0:3, :], XQ[u][0:3, q0:q1], start=False, stop=last2), reads=[onesb, XQ[u]], writes=[sp_])
                                else:
                                    S.op("pe", lambda e: e.matmul(sp_[:, c0:512], XK[u][:, ksl], XQ[u][:, q0:q1], start=False, stop=last2), reads=[XK[u], XQ[u]], writes=[sp_])
                                if j >= 0:
                                    S.op("pe", lambda e: e.matmul(sp_[:, c0:c0 + 128], identb[:], negtri[:], start=False, stop=True), reads=[identb, negtri], writes=[sp_])

                            def emit_P(idx):
                                g, kb = pairs[idx]
                                j = kb - 4 * g
                                c0 = max(j, 0) * 128
                                sp_ = SP[idx % 4]
                                pt = PT[idx % 4]
                                bias = nFk[u][:, kb:kb + 1] if kind == "fox" else zero_c[:]
                                S.op("act", lambda e: e.activation(pt[:, c0:512], sp_[:, c0:512], AF.Exp, bias=bias, scale=sc), reads=[sp_, nFk[u], zero_c], writes=[pt])
                                op_ = OP[g % 2]
                                rp_ = RP[g % 2]
                                lastk = 4 * g + 3
                                S.op("pe", lambda e: e.matmul(op_[:, c0:512], VV[u][:, kb, :], pt[:, c0:512], start=(kb == 0), stop=(kb == lastk)), reads=[VV[u], pt], writes=[op_])
                                S.op("pe", lambda e: e.matmul(rp_[:, c0:512], onesb[:], pt[:, c0:512], start=(kb == 0), stop=(kb == lastk)), reads=[onesb, pt], writes=[rp_])
                                if kb == lastk:
                                    S.op("dve", lambda e: e.reciprocal(rec[:], rp_[:]), reads=[rp_], writes=[rec])
                                    S.op("dve", lambda e: e.tensor_tensor(yTs[u][:, g * 512:(g + 1) * 512], op_[:], rec[:], ALU.mult), reads=[op_, rec], writes=[yTs[u]])

                            emit_S(0)
                            emit_S(1)
                            for idx in range(len(pairs)):
                                if idx + 2 < len(pairs):
                                    emit_S(idx + 2)
                                emit_P(idx)
                            S.dma("sp", yTd[ybr, h, :, s0:s0 + SEQ], yTs[u][:], reads=[yTs[u]])
                            if unit == 3:
                                wfill(Wg, [128, KC, 3 * D], Wl["Wg"], after=[yTs[u]])
                                wfill(Wbr, [128, 12, D], Wl["Wbr"], after=[yTs[u]])
                                wfill(Wo, [128, KC, D], Wl["Wo"], after=[yTs[u]])
                S.barrier()
            if stop_after == "B":
                cwst.close()
                lst.close()
                break

            ohst = contextlib.ExitStack()
            ohst.__enter__()
            OH1 = sbt(ohst, f"OH1_{l}", [128, NE, NT], F32)
            OH2 = sbt(ohst, f"OH2_{l}", [128, NE, NT], F32)
            lgA = sbt(ohst, f"lgA_{l}", [128, NT, 36], F32)
            with contextlib.ExitStack() as st:
                Wr = sbt(st, "Wr", [128, KC, 36], F32)
                brt = sbt(st, "brt", [128, 36], F32)
                S.dma("sp", Wr[:], Wl["Wr"], writes=[Wr])
                S.dma("sp", brt[:], Wl["br"], writes=[brt])
                h2f = sbt(st, "h2f", [128, KC, G], F32)
                htk = [sbt(st, f"htk{i}", [128, D], BF16) for i in range(2)]
                gb = sbt(st, "gb", [128, 24], F32)
                S.dma("sp", gb[:], Wl["gb"], writes=[gb])
                x32 = sbt(st, "x32c", [128, KC, G], F32)
                hT = sbt(st, "hTc", [128, KC, G], BF16)
                yT = sbt(st, "yTc", [128, 12, G], BF16)
                mT = sbt(st, "mTc", [128, KC, G], BF16)
                gate = [sbt(st, f"gate{i}", [128, G], F32) for i in range(3)]
                prod = [sbt(st, f"prod{i}", [128, G], F32) for i in range(3)]
                macc = sbt(st, "macc", [128, G], F32)
                for g in range(NG):
                    s = g // (NG // NS)
                    g0 = g * G
                    v = vec[l][s]
                    S.dma("sp", hT[:], hTd[:, :, g0:g0 + G].rearrange("k p t -> p k t"), writes=[hT])
                    for i in range(3):
                        S.dma("act", yT[:, i * 4:(i + 1) * 4, :], yTd[i, :, :, g0:g0 + G].rearrange("k p t -> p k t"), writes=[yT])
                    for half in range(2):
                        S.dma("sp" if half == 0 else "act", x32[:, half * 4:(half + 1) * 4, :],
                              xsrc[half * 4:(half + 1) * 4, :, g0:g0 + G].rearrange("k p t -> p k t"), writes=[x32])
                    for oc in range(KC):
                        for i in range(3):
                            pg = nps()
                            col = (i * 8 + oc) * 128
                            for kc in range(KC):
                                S.op("pe", lambda e: e.matmul(pg[:], Wg[:, kc, col:col + 128], hT[:, kc, :], start=(kc == 0), stop=(kc == KC - 1)), reads=[Wg, hT], writes=[pg])
                            S.op("act", lambda e: e.activation(gate[i][:], pg[:], AF.Sigmoid, bias=gb[:, i * 8 + oc:i * 8 + oc + 1], scale=1.0), reads=[pg, gb], writes=[gate[i]])
                            pb = nps()
                            for kc in range(4):
                                S.op("pe", lambda e: e.matmul(pb[:], Wbr[:, i * 4 + kc, oc * 128:(oc + 1) * 128], yT[:, i * 4 + kc, :], start=(kc == 0), stop=(kc == 3)), reads=[Wbr, yT], writes=[pb])
                            S.op("dve", lambda e: e.tensor_tensor(prod[i][:], pb[:], gate[i][:], ALU.mult), reads=[pb, gate[i]], writes=[prod[i]])
                        S.op("pool", lambda e: e.tensor_tensor(macc[:], prod[0][:], prod[1][:], ALU.add), reads=[prod[0], prod[1]], writes=[macc])
                        S.op("pool", lambda e: e.tensor_tensor(mT[:, oc, :], macc[:], prod[2][:], ALU.add), reads=[macc, prod[2]], writes=[mT])
                    for oc in range(KC):
                        po = nps()
                        for kc in range(KC):
                            S.op("pe", lambda e: e.matmul(po[:], Wo[:, kc, oc * 128:(oc + 1) * 128], mT[:, kc, :], start=(kc == 0), stop=(kc == KC - 1)), reads=[Wo, mT], writes=[po])
                        S.op("dve", lambda e: e.scalar_tensor_tensor(x32[:, oc, :], po[:], v[:, 2, oc:oc + 1], x32[:, oc, :], ALU.mult, ALU.add), reads=[po, x32], writes=[x32])
                    for half in range(2):
                        S.dma("sp" if half == 0 else "act", xw[half * 4:(half + 1) * 4, :, g0:g0 + G].rearrange("k p t -> p k t"),
                              x32[:, half * 4:(half + 1) * 4, :], reads=[x32])
                    norm_mod(x32, G, mT, prod[0], macc, lambda kc: v[:, 3, kc:kc + 1], lambda kc: v[:, 4, kc:kc + 1], out_f32=h2f)
                    for tt in range(4):
                        jt = g * 4 + tt
                        tsl = slice(tt * 128, (tt + 1) * 128)
                        hb = htk[jt % 2]
                        for half in range(2):
                            pst = nps()
                            for k4 in range(4):
                                kc = half * 4 + k4
                                S.op("pe", lambda e: e.transpose(pst[:, k4 * 128:(k4 + 1) * 128], h2f[:, kc, tsl], identf_ap), reads=[h2f, cst], writes=[pst])
                            if half == 0:
                                S.op("act", lambda e: e.activation(hb[:, 0:512], pst[:], AF.Copy), reads=[pst], writes=[hb])
                            else:
                                S.op("pool", lambda e: e.tensor_copy(hb[:, 512:1024], hb[:, 512:1024]), reads=[hb], writes=[hb]) if False else \
                                    S.op("act", lambda e: e.activation(hb[:, 512:1024], pst[:], AF.Copy), reads=[pst], writes=[hb])
                        S.dma("sp", h2tok[jt * 128:(jt + 1) * 128, :], hb[:], reads=[hb])
                        pl = nps()
                        for kc in range(KC):
                            S.op("pe", lambda e: e.matmul(pl[:, 0:36], h2f[:, kc, tsl], Wr[:, kc, :], start=(kc == 0), stop=(kc == KC - 1)), reads=[h2f, Wr], writes=[pl])
                        S.op("dve", lambda e: e.tensor_tensor(lgA[:, jt, :], pl[:, 0:36], brt[:], ALU.add), reads=[pl, brt], writes=[lgA])
                S.barrier()
            if stop_after == "C1":
                ohst.close()
                cwst.close()
                lst.close()
                break

            with contextlib.ExitStack() as st:
                r1 = sbt(st, "r1", [128, 5, NT], F32)
                ohgA = sbt(st, "ohgA", [128, NT, 4], F32)
                ex4A = sbt(st, "ex4A", [128, NT, 4], F32)
                ingA = sbt(st, "ingA", [128, NT, 8], F32)
                tm8 = sbt(st, "tm8", [128, NT, 8], F32)
                oh1A = sbt(st, "oh1A", [128, NT, 8], F32)
                oh2A = sbt(st, "oh2A", [128, NT, 8], F32)
                def bc(ap, shape):
                    return ap.to_broadcast(shape)
                g4 = lgA[:, :, 0:4]
                S.op("dve", lambda e: e.tensor_reduce(r1[:, 0, :], g4, AX.X, ALU.max), reads=[lgA], writes=[r1])
                S.op("dve", lambda e: e.tensor_tensor(ohgA[:], g4, bc(r1[:, 0, :].unsqueeze(2), [128, NT, 4]), ALU.is_equal), reads=[lgA, r1], writes=[ohgA])
                S.op("dve", lambda e: e.tensor_tensor(ex4A[:], g4, bc(r1[:, 0, :].unsqueeze(2), [128, NT, 4]), ALU.subtract), reads=[lgA, r1], writes=[ex4A])
                S.op("act", lambda e: e.activation(ex4A[:], ex4A[:], AF.Exp), reads=[ex4A], writes=[ex4A])
                S.op("dve", lambda e: e.tensor_reduce(r1[:, 1, :], ex4A[:], AX.X, ALU.add), reads=[ex4A], writes=[r1])
                S.op("dve", lambda e: e.reciprocal(r1[:, 1, :], r1[:, 1, :]), reads=[r1], writes=[r1])
                for gi in range(4):
                    dst = ingA if gi == 0 else tm8
                    S.op("dve", lambda e: e.tensor_tensor(dst[:], lgA[:, :, 4 + 8 * gi:12 + 8 * gi], bc(ohgA[:, :, gi:gi + 1], [128, NT, 8]), ALU.mult), reads=[lgA, ohgA], writes=[dst])
                    if gi > 0:
                        S.op("dve", lambda e: e.tensor_tensor(ingA[:], ingA[:], tm8[:], ALU.add), reads=[ingA, tm8], writes=[ingA])
                S.op("dve", lambda e: e.tensor_reduce(r1[:, 2, :], ingA[:], AX.X, ALU.max), reads=[ingA], writes=[r1])
                S.op("dve", lambda e: e.tensor_tensor(oh1A[:], ingA[:], bc(r1[:, 2, :].unsqueeze(2), [128, NT, 8]), ALU.is_equal), reads=[ingA, r1], writes=[oh1A])
                S.op("dve", lambda e: e.scalar_tensor_tensor(tm8[:], oh1A[:], -1e30, ingA[:], ALU.mult, ALU.add), reads=[oh1A, ingA], writes=[tm8])
                S.op("dve", lambda e: e.tensor_reduce(r1[:, 3, :], tm8[:], AX.X, ALU.max), reads=[tm8], writes=[r1])
                S.op("dve", lambda e: e.tensor_tensor(oh2A[:], tm8[:], bc(r1[:, 3, :].unsqueeze(2), [128, NT, 8]), ALU.is_equal), reads=[tm8, r1], writes=[oh2A])
                S.op("dve", lambda e: e.tensor_tensor(r1[:, 4, :], r1[:, 2, :], r1[:, 3, :], ALU.subtract), reads=[r1], writes=[r1])
                S.op("act", lambda e: e.activation(r1[:, 4, :], r1[:, 4, :], AF.Sigmoid), reads=[r1], writes=[r1])
                S.op("dve", lambda e: e.tensor_tensor(Wts[:, :, 0], r1[:, 4, :], r1[:, 1, :], ALU.mult), reads=[r1], writes=[Wts])
                S.op("dve", lambda e: e.tensor_tensor(Wts[:, :, 1], r1[:, 1, :], Wts[:, :, 0], ALU.subtract), reads=[r1, Wts], writes=[Wts])
                for gi in range(4):
                    gb_ = bc(ohgA[:, :, gi].unsqueeze(1), [128, 8, NT])
                    S.op("dve", lambda e: e.tensor_tensor(OH1[:, gi * 8:(gi + 1) * 8, :], oh1A[:].rearrange("p t j -> p j t"), gb_, ALU.mult), reads=[oh1A, ohgA], writes=[OH1])
                    S.op("dve", lambda e: e.tensor_tensor(OH2[:, gi * 8:(gi + 1) * 8, :], oh2A[:].rearrange("p t j -> p j t"), gb_, ALU.mult), reads=[oh2A, ohgA], writes=[OH2])
                S.barrier()

            with contextlib.ExitStack() as st:
                d1 = sbt(st, f"d1_{l}", [128, NT], I32)
                d2 = sbt(st, f"d2_{l}", [128, NT], I32)
                ib = sbt(st, f"ib_{l}", [128, NBLK], I32)
                NC2 = NE * NT
                Mb = sbt(st, "Mb", [128, NC2], BF16)
                Mf = sbt(st, "Mf", [128, NC2], F32)
                rank = sbt(st, "rank", [128, NC2], F32)
                tot = sbt(st, "tot", [128, NC2], F32)
                gin = sbt(st, "gin", [128, NC2], F32)
                onesN = sbt(st, "onesN", [128, NC2], F32)
                cnt = sbt(st, "cnt", [128, NE], F32)
                cnti = sbt(st, "cnti", [128, NE], I32)
                pcn = sbt(st, "pcn", [128, NE], F32)
                pin = sbt(st, "pin", [128, NE], F32)
                pof = sbt(st, "pof", [128, NE], F32)
                ones32 = sbt(st, "ones32", [128, NE], F32)
                be = sbt(st, "be", [128, NBLK], F32)
                bt = sbt(st, "bt", [128, NBLK], F32)
                eq = sbt(st, "eq", [128, NBLK], F32)
                df = sbt(st, "df", [128, NT], F32)
                o1f = OH1[:].rearrange("p e j -> p (e j)")
                o2f = OH2[:].rearrange("p e j -> p (e j)")
                S.op("dve", lambda e: e.memset(onesN[:], 1.0), writes=[onesN])
                S.op("dve", lambda e: e.memset(ones32[:], 1.0), writes=[ones32])
                S.op("dve", lambda e: e.tensor_tensor(Mf[:], o1f, o2f, ALU.add), reads=[OH1, OH2], writes=[Mf])
                S.op("dve", lambda e: e.tensor_copy(Mb[:], Mf[:]), reads=[Mf], writes=[Mb])
                for c in range(NC2 // 512):
                    cs = slice(c * 512, (c + 1) * 512)
                    p1 = nps()
                    S.op("pe", lambda e: e.matmul(p1[:], ustr[:], Mb[:, cs], start=True, stop=True), reads=[ustr, Mb], writes=[p1])
                    S.op("act", lambda e: e.activation(rank[:, cs], p1[:], AF.Copy), reads=[p1], writes=[rank])
                    p2 = nps()
                    S.op("pe", lambda e: e.matmul(p2[:], onesb[:], Mb[:, cs], start=True, stop=True), reads=[onesb, Mb], writes=[p2])
                    S.op("act", lambda e: e.activation(tot[:, cs], p2[:], AF.Copy), reads=[p2], writes=[tot])
                S.op("dve", lambda e: e.tensor_tensor_scan(gin[:], onesN[:], tot[:], 0.0, ALU.mult, ALU.add), reads=[onesN, tot], writes=[gin])
                S.op("dve", lambda e: e.tensor_tensor(gin[:], gin[:], tot[:], ALU.subtract), reads=[gin, tot], writes=[gin])
                S.op("dve", lambda e: e.tensor_reduce(cnt[:], tot[:].rearrange("p (e j) -> p e j", e=NE), AX.X, ALU.add), reads=[tot], writes=[cnt])
                S.op("dve", lambda e: e.tensor_scalar(cnti[:], cnt[:], 127.0, None, ALU.add), reads=[cnt], writes=[cnti])
                S.op("dve", lambda e: e.tensor_single_scalar(cnti[:], cnti[:], 7, ALU.arith_shift_right), reads=[cnti], writes=[cnti])
                S.op("dve", lambda e: e.tensor_copy(pcn[:], cnti[:]), reads=[cnti], writes=[pcn])
                S.op("dve", lambda e: e.tensor_scalar(pcn[:], pcn[:], 128.0, None, ALU.mult), reads=[pcn], writes=[pcn])
                S.op("dve", lambda e: e.tensor_tensor_scan(pin[:], ones32[:], pcn[:], 0.0, ALU.mult, ALU.add), reads=[ones32, pcn], writes=[pin])
                S.op("dve", lambda e: e.tensor_tensor(pof[:], pin[:], pcn[:], ALU.subtract), reads=[pin, pcn], writes=[pof])
                S.op("dve", lambda e: e.tensor_tensor(pof[:], pof[:], gin[:].rearrange("p (e j) -> p e j", e=NE)[:, :, 0], ALU.subtract), reads=[pof, gin], writes=[pof])
                S.op("dve", lambda e: e.tensor_tensor(gin[:], gin[:], rank[:], ALU.add), reads=[gin, rank], writes=[gin])
                for ee in range(NE):
                    S.op("dve", lambda e: e.tensor_scalar(gin[:, ee * NT:(ee + 1) * NT], gin[:, ee * NT:(ee + 1) * NT], pof[:, ee:ee + 1], None, ALU.add), reads=[gin, pof], writes=[gin])
                for k, (oh, dd) in enumerate(((o1f, d1), (o2f, d2))):
                    S.op("dve", lambda e: e.tensor_tensor(Mf[:], oh, gin[:], ALU.mult), reads=[OH1, OH2, gin], writes=[Mf])
                    S.op("dve", lambda e: e.tensor_reduce(df[:], Mf[:].rearrange("p (e j) -> p j e", e=NE), AX.X, ALU.add), reads=[Mf], writes=[df])
                    S.op("dve", lambda e: e.tensor_copy(dd[:], df[:]), reads=[df], writes=[dd])
                S.op("dve", lambda e: e.memset(be[:], 0.0), writes=[be])
                for ee in range(NE):
                    S.op("dve", lambda e: e.scalar_tensor_tensor(be[:], cst[:, C_THR:C_THR + NBLK], pin[:, ee:ee + 1], be[:], ALU.is_ge, ALU.add), reads=[cst, pin, be], writes=[be])
                S.op("dve", lambda e: e.tensor_scalar(be[:], be[:], float(NE - 1), None, ALU.min), reads=[be], writes=[be])
                S.op("dve", lambda e: e.memset(eq[:], 0.0), writes=[eq])
                S.op("dve", lambda e: e.tensor_tensor(eq[:, 3:NBLK], be[:, 3:NBLK], be[:, 0:NBLK - 3], ALU.is_equal), reads=[be], writes=[eq])
                S.op("dve", lambda e: e.tensor_scalar(bt[:], be[:], 128.0, cst[:, C_IOTA:C_IOTA + 1], ALU.mult, ALU.add), reads=[be, cst], writes=[bt])
                S.op("dve", lambda e: e.scalar_tensor_tensor(bt[:], eq[:], SKIP_OFF, bt[:], ALU.mult, ALU.add), reads=[eq, bt], writes=[bt])
                S.op("dve", lambda e: e.tensor_copy(ib[:], bt[:]), reads=[bt], writes=[ib])
                for nm, src, dst in (("d1", d1, d1v), ("d2", d2, d2v), ("ib", ib, ibv)):
                    shp = [128] + [int(x) for x in list(src.ap.shape)[1:]]
                    scr = nc.dram_tensor(f"ix_{nm}_{l}", shp, I32, kind="Internal").ap()
                    S.dma("sp", scr, src[:], reads=[src])
                    S.barrier()
                    S.dma("sp", dst[:], scr, writes=[dst])
                if "rdbg" in dbg:
                    S.op("dve", lambda e: e.tensor_copy(Mf[:, 0:NT], d1[:]), reads=[d1], writes=[Mf])
                    S.op("dve", lambda e: e.tensor_copy(Mf[:, NT:2 * NT], d2[:]), reads=[d2], writes=[Mf])
                    S.op("dve", lambda e: e.tensor_copy(Mf[:, 2 * NT:4 * NT], Wts[:].rearrange("p j k -> p (j k)")), reads=[Wts], writes=[Mf])
                    S.dma("sp", rdbg, Mf[:, 0:4 * NT], reads=[Mf])
                S.barrier()
            ohst.close()
            cwst.close()
            if stop_after == "C2":
                lst.close()
                break

            xs = nc.dram_tensor(f"xs_l{l}", [NSLOT, D], BF16, kind="Internal").ap()
            Ys = nc.dram_tensor(f"Ys_l{l}", [NSLOT, D], F32, kind="Internal").ap()
            with contextlib.ExitStack() as st:
                hb = [sbt(st, f"hbs{i}", [128, D], BF16) for i in range(3)]
                for jt in range(NT):
                    b = hb[jt % 3]
                    S.dma("sp", b[:], h2tok[jt * 128:(jt + 1) * 128, :], writes=[b])
                    for dd in (d1v, d2v):
                        S.dma("pool", None, None, reads=[b, dd], fn=lambda e: e.indirect_dma_start(
                            out=xs, out_offset=bass.IndirectOffsetOnAxis(ap=dd[:, jt:jt + 1], axis=0), in_=b[:], in_offset=None,
                            bounds_check=r_slot, oob_is_err=False))
                S.barrier()

            if stop_after == "D1":
                lst.close()
                break
            with contextlib.ExitStack() as st:
                wcb = [sbt(st, f"wcb_{i}", [128, 12288], BF16) for i in range(3)]
                xb = [sbt(st, f"xb{i}", [128, D], BF16) for i in range(3)]
                xbT = [sbt(st, f"xbT{i}", [128, KC, 128], BF16) for i in range(2)]
                sil = [sbt(st, f"sil{i}", [128, 512], F32) for i in range(2)]
                hid = [sbt(st, f"hid{i}", [128, 512], BF16) for i in range(2)]
                hidT = [sbt(st, f"hidT{i}", [128, 4, 128], BF16) for i in range(2)]
                ysb = [sbt(st, f"ysb{i}", [128, D], F32) for i in range(2)]

                def d2_s1a(b):
                    u = b % 2
                    w = b % 3
                    S.dma("pool", None, None, reads=[ibv], writes=[wcb[w]], fn=lambda e: e.indirect_dma_start(
                        out=wcb[w][:], out_offset=None, in_=Wcb, in_offset=bass.IndirectOffsetOnAxis(ap=ibv[:, b:b + 1], axis=0),
                        bounds_check=r_e, oob_is_err=False))
                    xbb = xb[b % 3]
                    S.dma("sp", xbb[:], xs[b * 128:(b + 1) * 128, :], writes=[xbb])
                    pst = PS[u]
                    pstb = pst[:].bitcast(BF16)
                    for kc in range(KC):
                        S.op("pe", lambda e: e.transpose(pstb[:, kc * 128:(kc + 1) * 128], xbb[:, kc * 128:(kc + 1) * 128], identb[:]), reads=[xbb, identb], writes=[pst])
                    xv = xbT[u][:].rearrange("p k t -> p (k t)")
                    S.op("act", lambda e: e.activation(xv[:, 0:512], pstb[:, 0:512], AF.Copy), reads=[pst], writes=[xbT[u]])
                    S.op("dve", lambda e: e.tensor_copy(xv[:, 512:1024], pstb[:, 512:1024]), reads=[pst], writes=[xbT[u]])

                def d2_s1b(b):
                    u = b % 2
                    w = b % 3
                    w13v = wcb[w][:, 0:8192].rearrange("p (a c) -> p a c", c=512)
                    p1 = PS[3]
                    p3 = PS[4]
                    for kc in range(KC):
                        S.op("pe", lambda e: e.matmul(p1[:], xbT[u][:, kc, :], w13v[:, kc, :], start=(kc == 0), stop=(kc == KC - 1)), reads=[xbT[u], wcb[w]], writes=[p1])
                        S.op("pe", lambda e: e.matmul(p3[:], xbT[u][:, kc, :], w13v[:, 8 + kc, :], start=(kc == 0), stop=(kc == KC - 1)), reads=[xbT[u], wcb[w]], writes=[p3])
                    S.op("act", lambda e: e.activation(sil[u][:], p1[:], AF.Silu), reads=[p1], writes=[sil[u]])
                    S.op("dve", lambda e: e.tensor_tensor(hid[u][:], p3[:], sil[u][:], ALU.mult), reads=[p3, sil[u]], writes=[hid[u]])

                def d2_s2a(b):
                    u = b % 2
                    pt2 = PS[2]
                    pt2b = pt2[:].bitcast(BF16)
                    for kc in range(4):
                        S.op("pe", lambda e: e.transpose(pt2b[:, kc * 128:(kc + 1) * 128], hid[u][:, kc * 128:(kc + 1) * 128], identb[:]), reads=[hid[u], identb], writes=[pt2])
                    S.op("act", lambda e: e.activation(hidT[u][:].rearrange("p k t -> p (k t)"), pt2b[:, 0:512], AF.Copy), reads=[pt2], writes=[hidT[u]])

                def d2_s2b(b):
                    u = b % 2
                    w = b % 3
                    w2v = wcb[w][:, 8192:12288].rearrange("p (a c) -> p a c", c=1024)
                    for half in range(2):
                        py = PS[5 + half]
                        for kc in range(4):
                            S.op("pe", lambda e: e.matmul(py[:], hidT[u][:, kc, :], w2v[:, kc, half * 512:(half + 1) * 512], start=(kc == 0), stop=(kc == 3)), reads=[hidT[u], wcb[w]], writes=[py])
                        if half == 0:
                            S.op("act", lambda e: e.activation(ysb[u][:, 0:512], py[:], AF.Copy), reads=[py], writes=[ysb[u]])
                        else:
                            S.op("dve", lambda e: e.tensor_copy(ysb[u][:, 512:1024], py[:]), reads=[py], writes=[ysb[u]])
                    S.dma("sp", Ys[b * 128:(b + 1) * 128, :], ysb[u][:], reads=[ysb[u]])

                d2_s1a(0)
                d2_s1a(1)
                d2_s1b(0)
                for b in range(NBLK):
                    if b + 2 < NBLK:
                        d2_s1a(b + 2)
                    d2_s2a(b)
                    if b + 1 < NBLK:
                        d2_s1b(b + 1)
                    d2_s2b(b)
                S.barrier()
            if stop_after == "D2":
                lst.close()
                break
            last = (l == nlayers - 1)
            with contextlib.ExitStack() as st:
                NBE = 4
                y1 = [sbt(st, f"y1_{i}", [128, D], F32) for i in range(NBE)]
                y2 = [sbt(st, f"y2_{i}", [128, D], F32) for i in range(NBE)]
                cmb = [sbt(st, f"cmb{i}", [128, D], F32) for i in range(NBE)]
                xt = [sbt(st, f"xt{i}", [128, KC, 128], F32) for i in range(NBE)]
                sqe = sbt(st, "sqe", [128, KC, 128], BF16)
                rse = sbt(st, "rse", [128, 128], F32)
                tme = sbt(st, "tme", [128, 128], F32)
                xo = [sbt(st, f"xo{i}", [128, KC, 128], F32) for i in range(NBE)]
                for jt in range(NT):
                    u = jt % NBE
                    s = jt // (NT // NS)
                    v = vec[l][s]
                    tk = slice(jt * 128, (jt + 1) * 128)
                    S.dma("sp", xt[u][:], xw[:, :, tk].rearrange("k p t -> p k t"), writes=[xt[u]])
                    for yy, dd in ((y1[u], d1v), (y2[u], d2v)):
                        S.dma("pool", None, None, reads=[dd], writes=[yy], fn=lambda e: e.indirect_dma_start(
                            out=yy[:], out_offset=None, in_=Ys, in_offset=bass.IndirectOffsetOnAxis(ap=dd[:, jt:jt + 1], axis=0),
                            bounds_check=r_slot, oob_is_err=False))
                    S.op("dve", lambda e: e.tensor_scalar(cmb[u][:], y1[u][:], Wts[:, jt, 0:1], None, ALU.mult), reads=[y1[u], Wts], writes=[cmb[u]])
                    S.op("dve", lambda e: e.scalar_tensor_tensor(cmb[u][:], y2[u][:], Wts[:, jt, 1:2], cmb[u][:], ALU.mult, ALU.add), reads=[y2[u], Wts, cmb[u]], writes=[cmb[u]])
                    for half in range(2):
                        pt = nps()
                        for k4 in range(4):
                            kc = half * 4 + k4
                            S.op("pe", lambda e: e.transpose(pt[:, k4 * 128:(k4 + 1) * 128], cmb[u][:, kc * 128:(kc + 1) * 128], identf_ap), reads=[cmb[u], cst], writes=[pt])
                        for k4 in range(4):
                            kc = half * 4 + k4
                            S.op("dve", lambda e: e.scalar_tensor_tensor(xt[u][:, kc, :], pt[:, k4 * 128:(k4 + 1) * 128], v[:, 5, kc:kc + 1], xt[u][:, kc, :], ALU.mult, ALU.add), reads=[pt, xt[u]], writes=[xt[u]])
                    if not last:
                        S.dma("act", xw[:, :, tk].rearrange("k p t -> p k t"), xt[u][:], reads=[xt[u]])
                    else:
                        S.op("act", lambda e: e.activation(sqe[:], xt[u][:], AF.Square), reads=[xt[u]], writes=[sqe])
                        ssp = nps()
                        for kc in range(KC):
                            S.op("pe", lambda e: e.matmul(ssp[:, 0:128], onesb[:], sqe[:, kc, :], start=(kc == 0), stop=(kc == KC - 1)), reads=[onesb, sqe], writes=[ssp])
                        S.op("act", lambda e: e.activation(rse[:], ssp[:, 0:128], AF.Sqrt, bias=epsc[:], scale=1.0 / D), reads=[ssp, epsc], writes=[rse])
                        S.op("dve", lambda e: e.reciprocal(rse[:], rse[:]), reads=[rse], writes=[rse])
                        for kc in range(KC):
                            S.op("dve", lambda e: e.scalar_tensor_tensor(xo[u][:, kc, :], xt[u][:, kc, :], fgt[:, kc:kc + 1], rse[:], ALU.mult, ALU.mult), reads=[xt[u], fgt, rse], writes=[xo[u]])
                        S.dma("act", outT[:, :, tk].rearrange("k p t -> p k t"), xo[u][:], reads=[xo[u]])
                S.barrier()
            lst.close()
        S.barrier()
        print("bass program: ninst", S.ninst, S.cnt, flush=True)
    return nc


def _kc(a):
    k = a.shape[0] // 128
    return np.ascontiguousarray(a.reshape(k, 128, *a.shape[1:]).swapaxes(0, 1))


def make_consts():
    c = np.zeros((128, C_N), np.float32)
    c[:, C_ID:C_ID + 128] = np.eye(128, dtype=np.float32)
    k = np.arange(128)[:, None]
    q = np.arange(128)[None, :]
    c[:, C_TRI:C_TRI + 128] = np.where(k <= q, 0.0, -1e9)
    c[:, C_US:C_US + 128] = (k < q).astype(np.float32)
    for h in range(4):
        lg = np.log1p(-2.0 ** (-5.0 - h))
        rel = (q - k).astype(np.float64)
        dm = np.where(rel >= 0, np.exp(lg * np.maximum(rel, 0.0)), 0.0) * 0.125
        c[:, C_DM + h * 128:C_DM + (h + 1) * 128] = dm
        c[:, C_DI + h * 128:C_DI + (h + 1) * 128] = np.exp(lg * (np.arange(128) + 1.0))[None, :]
        c[:, C_DO + h] = np.exp(lg * (127.0 - np.arange(128))) * 0.125
    inv = (10000.0 ** (-np.arange(0, 64, 2, dtype=np.float32) / 64)).astype(np.float32)
    r = np.arange(128)
    c[:, C_INVF] = inv[r % 32]
    c[:, C_SGN] = np.where((r % 64) < 32, -1.0, 1.0)
    c[:, C_IOTA] = r
    c[:, C_THR:C_THR + NBLK] = (128.0 * np.arange(NBLK))[None, :]
    return c


def _swap64(a):
    n = a.shape[1] // 64
    b = a.reshape(a.shape[0], n, 2, 32)[:, :, ::-1, :]
    return b.reshape(a.shape[0], n * 64)


def prep_shared(inp):
    sh = {"consts": make_consts(), "fg": np.ascontiguousarray(inp["final_g"].reshape(KC, 128).T)}
    f32 = np.float32
    for l in range(L):
        w_in = inp["w_in"][l]
        o = np.cumsum([0, 256, 256, 512, 512, 512, 512, 512, 4, 256, 128, 64])
        rq, rk, rv, rg, fq, fk, fv, ff, mq, mkv, mkr = [w_in[:, o[i]:o[i + 1]] for i in range(11)]
        WA = np.concatenate([rq, _swap64(rq), rk, _swap64(rk), fq, fk, mq, mkv, mkr, _swap64(mkr), ff], axis=1)
        WB = np.concatenate([rv, rg, fv], axis=1)
        assert WA.shape[1] == WA_COLS and WB.shape[1] == WB_COLS
        wq = inp["mla_wq_up"][l].reshape(256, 4, 192)
        qn = wq[:, :, :128].reshape(256, 512)
        qp = wq[:, :, 128:].reshape(256, 256)
        wqc = np.concatenate([qn, qp, _swap64(qp)], axis=1)
        wkv = inp["mla_wkv_up"][l].reshape(128, 4, 256)
        wkvc = np.concatenate([wkv[:, :, :128].reshape(128, 512), wkv[:, :, 128:].reshape(128, 512)], axis=1)
        w1 = inp["exp_w1"][l].reshape(NE, KC, 128, 512).transpose(0, 2, 1, 3)
        w3 = inp["exp_w3"][l].reshape(NE, KC, 128, 512).transpose(0, 2, 1, 3)
        W13 = np.stack([w1, w3], axis=2)
        W2 = inp["exp_w2"][l].reshape(NE, 4, 128, 1024).transpose(0, 2, 1, 3)
        sh.update({
            f"adaw{l}": _kc(inp["ada_w"][l]), f"adab{l}": np.ascontiguousarray(inp["ada_b"][l].reshape(48, 128).T),
            f"n1g{l}": np.ascontiguousarray(inp["norm1_g"][l].reshape(KC, 128).T), f"n2g{l}": np.ascontiguousarray(inp["norm2_g"][l].reshape(KC, 128).T),
            f"WA{l}": _kc(WA), f"WB{l}": _kc(WB), f"fb{l}": np.ascontiguousarray(inp["fox_fb"][l].reshape(4, 1)),
            f"gq{l}": np.ascontiguousarray(inp["mla_q_norm_g"][l].reshape(2, 128).T), f"wq{l}": _kc(wqc),
            f"gkv{l}": np.ascontiguousarray(inp["mla_kv_norm_g"][l].reshape(128, 1)), f"wkv{l}": np.ascontiguousarray(wkvc),
            f"Wg{l}": _kc(inp["gate_w"][l]), f"gb{l}": np.ascontiguousarray(inp["gate_b"][l].reshape(24, 128).T),
            f"Wbr{l}": np.ascontiguousarray(inp["branch_w"][l].reshape(12, 128, D).swapaxes(0, 1)),
            f"Wo{l}": _kc(inp["out_w"][l]),
            f"Wr{l}": _kc(np.concatenate([inp["router_grp_w"][l], inp["router_exp_w"][l]], axis=1)),
            f"br{l}": np.ascontiguousarray(np.broadcast_to(np.concatenate([inp["router_grp_b"][l], inp["router_exp_b"][l]])[None, :], (128, 36))),
            f"W13{l}": np.ascontiguousarray(W13).reshape(NE * 128 * 4, 2048),
            f"W2{l}": np.ascontiguousarray(W2).reshape(NE * 128 * 2, 2048),
        })
    return {k: np.ascontiguousarray(v, dtype=f32) for k, v in sh.items()}


def prep_core(inp, core):
    b0 = core * NS
    x = inp["x"][b0:b0 + NS]
    xT = np.ascontiguousarray(x.transpose(2, 0, 1).reshape(KC, 128, T))
    c = inp["c"][b0:b0 + NS]
    cT = np.ascontiguousarray(c.reshape(NS, KC, 128).transpose(2, 1, 0))
    pos = np.ascontiguousarray(inp["positions"][b0:b0 + NS].reshape(1, T).astype(np.int32))
    return {"xT": xT.astype(np.float32), "cT": cT.astype(np.float32), "pos": pos}


def kernel(**inputs):
    inp = {k: np.asarray(v) for k, v in inputs.items()}
    shared = prep_shared(inp)
    nc = build()
    in_maps = []
    for c in range(NCORE):
        m = dict(shared)
        m.update(prep_core(inp, c))
        in_maps.append(m)
    res = run_bass_kernel_spmd(nc, in_maps, core_ids=list(range(NCORE)))
    out = np.empty((NCORE * NS, SEQ, D), np.float32)
    for c in range(NCORE):
        o = np.asarray(res.results[c]["outT"]).reshape(D, NS, SEQ)
        out[c * NS:(c + 1) * NS] = o.transpose(1, 2, 0)
    return out
```
